# Optimizing a Trainium2 kernel written in Bass

```python
import math
import jax
import jax.numpy as jnp
from jax import lax
import numpy as np


D_MODEL = 1024
BATCH = 4
SEQ = 8192
DEPTH = 1

S5_WIDTH = D_MODEL // 2
S5_GROUP = 16
S5_GROUPS = S5_WIDTH // S5_GROUP
S5_STATE = 64
S5_DT_MIN = 0.001
S5_DT_MAX = 0.1
SSD_WIDTH = D_MODEL
SSD_HEADDIM = 64
SSD_HEADS = SSD_WIDTH // SSD_HEADDIM
SSD_GROUPS = 4
SSD_STATE = 64
SSD_CONV = 5
SSD_CHUNK = 128
SSD_CONV_DIM = SSD_WIDTH + 2 * SSD_GROUPS * SSD_STATE
PEER_HEADS = 8
PEER_NKEYS = 128
PEER_EXPERTS = PEER_NKEYS * PEER_NKEYS
PEER_DKEY = 256
PEER_TOPK = 16
PEER_BLOCK = 128
IN_WIDTH = S5_WIDTH + SSD_WIDTH + SSD_CONV_DIM + 2 * SSD_HEADS + 2 * D_MODEL
IN_SPLITS = (S5_WIDTH,
             S5_WIDTH + SSD_WIDTH,
             S5_WIDTH + SSD_WIDTH + SSD_CONV_DIM,
             S5_WIDTH + SSD_WIDTH + SSD_CONV_DIM + 2 * SSD_HEADS,
             S5_WIDTH + SSD_WIDTH + SSD_CONV_DIM + 2 * SSD_HEADS + D_MODEL)
RMS_EPS = 1e-6

kernel_name = 'hybrid_s5_ssd_peer_encoder'


def rmsnorm(x, w):
    xf = x.astype(jnp.float32)
    return xf * lax.rsqrt(jnp.mean(xf * xf, axis=-1, keepdims=True) + RMS_EPS) * w.astype(jnp.float32)


def _cplx_combine(e_i, e_j):
    ar_i, ai_i, br_i, bi_i = e_i
    ar_j, ai_j, br_j, bi_j = e_j
    return (ar_j * ar_i - ai_j * ai_i,
            ar_j * ai_i + ai_j * ar_i,
            ar_j * br_i - ai_j * bi_i + br_j,
            ar_j * bi_i + ai_j * br_i + bi_j)


def s5_bidirectional(u, a_re, a_im, log_dt, b_re, b_im, c_re, c_im, d_skip):
    bsz, seqlen, _ = u.shape
    ug = u.reshape(bsz, seqlen, S5_GROUPS, S5_GROUP)
    y = u * d_skip
    for direction in range(2):
        ar = a_re[direction].astype(jnp.float32)
        ai = a_im[direction].astype(jnp.float32)
        step = jnp.exp(log_dt[direction].astype(jnp.float32))[:, None]
        mag = jnp.exp(ar * step)
        lam_r = mag * jnp.cos(ai * step)
        lam_i = mag * jnp.sin(ai * step)
        den = ar * ar + ai * ai
        f_r = ((lam_r - 1.0) * ar + lam_i * ai) / den
        f_i = (lam_i * ar - (lam_r - 1.0) * ai) / den
        bu_r0 = jnp.einsum('blgc,gpc->blgp', ug, b_re[direction])
        bu_i0 = jnp.einsum('blgc,gpc->blgp', ug, b_im[direction])
        bu_r = f_r * bu_r0 - f_i * bu_i0
        bu_i = f_r * bu_i0 + f_i * bu_r0
        lr = jnp.broadcast_to(lam_r, (1, seqlen) + lam_r.shape)
        li = jnp.broadcast_to(lam_i, (1, seqlen) + lam_i.shape)
        _, _, s_r, s_i = lax.associative_scan(_cplx_combine, (lr, li, bu_r, bu_i),
                                              reverse=(direction == 1), axis=1)
        y_dir = (jnp.einsum('blgp,gcp->blgc', s_r, c_re[direction])
                 - jnp.einsum('blgp,gcp->blgc', s_i, c_im[direction]))
        y = y + y_dir.reshape(bsz, seqlen, S5_WIDTH)
    return y


def _segsum_exp(cs):
    t = cs.shape[-1]
    diff = cs[..., :, None] - cs[..., None, :]
    mask = jnp.tril(jnp.ones((t, t), dtype=bool))
    return jnp.exp(jnp.where(mask, diff, -jnp.inf))


def ssd_scan(xh, dt, a, bg, cg):
    bsz, seqlen = xh.shape[:2]
    nc = seqlen // SSD_CHUNK
    hg = SSD_HEADS // SSD_GROUPS
    xdt = (xh * dt[..., None]).reshape(bsz, nc, SSD_CHUNK, SSD_GROUPS, hg, SSD_HEADDIM)
    a_dt = jnp.moveaxis((dt * a).reshape(bsz, nc, SSD_CHUNK, SSD_GROUPS, hg), 2, -1)
    a_cs = jnp.cumsum(a_dt, axis=-1)
    bc = bg.reshape(bsz, nc, SSD_CHUNK, SSD_GROUPS, SSD_STATE)
    cc = cg.reshape(bsz, nc, SSD_CHUNK, SSD_GROUPS, SSD_STATE)
    decay_in = _segsum_exp(a_cs)
    cb = jnp.einsum('bclgn,bcsgn->bcgls', cc, bc)
    y_diag = jnp.einsum('bcghls,bcsghp->bclghp', cb[:, :, :, None] * decay_in, xdt)
    decay_states = jnp.exp(a_cs[..., -1:] - a_cs)
    states = jnp.einsum('bclgn,bcghl,bclghp->bcghpn', bc, decay_states, xdt)
    chunk_tot = jnp.moveaxis(a_cs[..., -1], 1, -1)
    chunk_cs = jnp.cumsum(jnp.pad(chunk_tot, ((0, 0), (0, 0), (0, 0), (1, 0))), axis=-1)
    decay_chunk = _segsum_exp(chunk_cs)
    states_p = jnp.concatenate([jnp.zeros_like(states[:, :1]), states], axis=1)
    new_states = jnp.einsum('bghzc,bcghpn->bzghpn', decay_chunk, states_p)
    prev_states = new_states[:, :-1]
    y_off = jnp.einsum('bclgn,bcghpn,bcghl->bclghp', cc, prev_states, jnp.exp(a_cs))
    return (y_diag + y_off).reshape(bsz, seqlen, SSD_HEADS, SSD_HEADDIM)


def ssd_bidirectional(z, xbc, dt_raw, conv_w, conv_b, dt_bias, a_log, d_skip, norm_w, w_out):
    bsz, seqlen, _ = xbc.shape
    pad = SSD_CONV // 2
    xbc = lax.conv_general_dilated(xbc, conv_w.astype(xbc.dtype), window_strides=(1,),
                                   padding=[(pad, pad)],
                                   dimension_numbers=('NWC', 'WIO', 'NWC'),
                                   feature_group_count=SSD_CONV_DIM)
    xbc = jax.nn.silu(xbc + conv_b)
    xs, bs, cs = jnp.split(xbc, (SSD_WIDTH, SSD_WIDTH + SSD_GROUPS * SSD_STATE), axis=-1)
    xh = xs.reshape(bsz, seqlen, SSD_HEADS, SSD_HEADDIM)
    bg = bs.reshape(bsz, seqlen, SSD_GROUPS, SSD_STATE)
    cg = cs.reshape(bsz, seqlen, SSD_GROUPS, SSD_STATE)
    y = xh * d_skip[:, None]
    for direction in range(2):
        dt = jax.nn.softplus(dt_raw[..., direction * SSD_HEADS:(direction + 1) * SSD_HEADS].astype(jnp.float32)
                             + dt_bias[direction])
        a = -jnp.exp(a_log[direction].astype(jnp.float32))
        if direction == 0:
            y = y + ssd_scan(xh, dt, a, bg, cg)
        else:
            y_b = ssd_scan(jnp.flip(xh, 1), jnp.flip(dt, 1), a, jnp.flip(bg, 1), jnp.flip(cg, 1))
            y = y + jnp.flip(y_b, 1)
    y = y.reshape(bsz, seqlen, SSD_WIDTH) * jax.nn.silu(z)
    yg = y.reshape(bsz, seqlen, SSD_GROUPS, SSD_WIDTH // SSD_GROUPS).astype(jnp.float32)
    yg = yg * lax.rsqrt(jnp.mean(yg * yg, axis=-1, keepdims=True) + RMS_EPS)
    y = yg.reshape(bsz, seqlen, SSD_WIDTH) * norm_w
    return y @ w_out


def peer_ffn(xn, w_q, sub_keys, u_tab, v_tab):
    bsz, seqlen, d = xn.shape
    t = bsz * seqlen
    xt = xn.reshape(t, d)
    q = (xt @ w_q).reshape(t, PEER_HEADS, 2, PEER_DKEY // 2).astype(jnp.float32)
    scores = jnp.einsum('thsd,hskd->thsk', q, sub_keys.astype(jnp.float32))
    top_s, top_i = lax.top_k(scores, PEER_TOPK)
    cand = (top_s[:, :, 0, :, None] + top_s[:, :, 1, None, :]).reshape(t, PEER_HEADS, PEER_TOPK * PEER_TOPK)
    best_s, best_c = lax.top_k(cand, PEER_TOPK)
    i1 = jnp.take_along_axis(top_i[:, :, 0], best_c // PEER_TOPK, axis=-1)
    i2 = jnp.take_along_axis(top_i[:, :, 1], best_c % PEER_TOPK, axis=-1)
    expert = (i1 * PEER_NKEYS + i2).reshape(t, PEER_HEADS * PEER_TOPK)
    gates = jax.nn.softmax(best_s, axis=-1).reshape(t, PEER_HEADS * PEER_TOPK)
    nb = t // PEER_BLOCK

    def block(args):
        xb, eb, gb = args
        hid = jnp.einsum('td,tkd->tk', xb, u_tab[eb])
        act = jax.nn.gelu(hid, approximate=False) * gb
        return jnp.einsum('tk,tkd->td', act, v_tab[eb])

    out = lax.map(block, (xt.reshape(nb, PEER_BLOCK, d),
                          expert.reshape(nb, PEER_BLOCK, PEER_HEADS * PEER_TOPK),
                          gates.reshape(nb, PEER_BLOCK, PEER_HEADS * PEER_TOPK)))
    return out.reshape(bsz, seqlen, d)


def setup_inputs(seed: int = 0) -> dict:
    key = jax.random.key(seed)
    ks = jax.random.split(key, 26)
    f32 = jnp.float32

    def nrm(k, shape, scale):
        return jax.random.normal(k, shape, f32) * scale

    def gain(k, shape):
        return 1.0 + 0.01 * jax.random.normal(k, shape, f32)

    nl = DEPTH
    x = nrm(ks[0], (BATCH, SEQ, D_MODEL), 1.0)
    mix_norm_w = gain(ks[1], (nl, D_MODEL))
    w_in = nrm(ks[2], (nl, D_MODEL, IN_WIDTH), D_MODEL ** -0.5)
    s5_a_re = -0.5 + nrm(ks[3], (nl, 2, S5_GROUPS, S5_STATE), 0.01)
    s5_a_im = math.pi * jnp.arange(S5_STATE, dtype=f32) + nrm(ks[4], (nl, 2, S5_GROUPS, S5_STATE), 0.01)
    s5_log_dt = jax.random.uniform(ks[5], (nl, 2, S5_GROUPS), f32, math.log(S5_DT_MIN), math.log(S5_DT_MAX))
    s5_b_re = nrm(ks[6], (nl, 2, S5_GROUPS, S5_STATE, S5_GROUP), (2 * S5_GROUP) ** -0.5)
    s5_b_im = nrm(ks[7], (nl, 2, S5_GROUPS, S5_STATE, S5_GROUP), (2 * S5_GROUP) ** -0.5)
    s5_c_re = nrm(ks[8], (nl, 2, S5_GROUPS, S5_GROUP, S5_STATE), S5_STATE ** -0.5)
    s5_c_im = nrm(ks[9], (nl, 2, S5_GROUPS, S5_GROUP, S5_STATE), S5_STATE ** -0.5)
    s5_d = nrm(ks[10], (nl, S5_WIDTH), 1.0)
    w_glu = nrm(ks[11], (nl, S5_WIDTH, 2 * D_MODEL), S5_WIDTH ** -0.5)
    conv_w = nrm(ks[12], (nl, SSD_CONV, 1, SSD_CONV_DIM), SSD_CONV ** -0.5)
    conv_b = nrm(ks[13], (nl, SSD_CONV_DIM), 0.02)
    dt0 = jnp.exp(jax.random.uniform(ks[14], (nl, 2, SSD_HEADS), f32, math.log(1e-3), math.log(1e-1)))
    ssd_dt_bias = dt0 + jnp.log(-jnp.expm1(-dt0))
    ssd_a_log = jnp.log(jax.random.uniform(ks[15], (nl, 2, SSD_HEADS), f32, 1.0, 16.0))
    ssd_d = gain(ks[16], (nl, SSD_HEADS))
    ssd_norm_w = gain(ks[17], (nl, SSD_WIDTH))
    w_ssd_out = nrm(ks[18], (nl, SSD_WIDTH, D_MODEL), SSD_WIDTH ** -0.5)
    w_o = nrm(ks[19], (nl, D_MODEL, D_MODEL), D_MODEL ** -0.5)
    ffn_norm_w = gain(ks[20], (nl, D_MODEL))
    peer_w_q = nrm(ks[21], (nl, D_MODEL, PEER_HEADS * PEER_DKEY), D_MODEL ** -0.5)
    peer_sub_keys = nrm(ks[22], (nl, PEER_HEADS, 2, PEER_NKEYS, PEER_DKEY // 2), (PEER_DKEY // 2) ** -0.5)
    peer_u = nrm(ks[23], (nl, PEER_EXPERTS, D_MODEL), D_MODEL ** -0.5)
    peer_v = nrm(ks[24], (nl, PEER_EXPERTS, D_MODEL), PEER_HEADS ** -0.5)
    final_norm_w = gain(ks[25], (D_MODEL,))
    return {'x': x, 'mix_norm_w': mix_norm_w, 'w_in': w_in,
            's5_a_re': s5_a_re, 's5_a_im': s5_a_im, 's5_log_dt': s5_log_dt,
            's5_b_re': s5_b_re, 's5_b_im': s5_b_im, 's5_c_re': s5_c_re, 's5_c_im': s5_c_im,
            's5_d': s5_d, 'w_glu': w_glu, 'conv_w': conv_w, 'conv_b': conv_b,
            'ssd_dt_bias': ssd_dt_bias, 'ssd_a_log': ssd_a_log, 'ssd_d': ssd_d,
            'ssd_norm_w': ssd_norm_w, 'w_ssd_out': w_ssd_out, 'w_o': w_o,
            'ffn_norm_w': ffn_norm_w, 'peer_w_q': peer_w_q, 'peer_sub_keys': peer_sub_keys,
            'peer_u': peer_u, 'peer_v': peer_v, 'final_norm_w': final_norm_w}


def reference(x, mix_norm_w, w_in, s5_a_re, s5_a_im, s5_log_dt, s5_b_re, s5_b_im, s5_c_re, s5_c_im,
              s5_d, w_glu, conv_w, conv_b, ssd_dt_bias, ssd_a_log, ssd_d, ssd_norm_w, w_ssd_out, w_o,
              ffn_norm_w, peer_w_q, peer_sub_keys, peer_u, peer_v, final_norm_w):
    h = x
    for layer in range(DEPTH):
        xn = rmsnorm(h, mix_norm_w[layer])
        proj = xn @ w_in[layer]
        u_s5, z, xbc, dt_raw, gate_a, gate_b = jnp.split(proj, IN_SPLITS, axis=-1)
        y_a = s5_bidirectional(u_s5, s5_a_re[layer], s5_a_im[layer], s5_log_dt[layer],
                               s5_b_re[layer], s5_b_im[layer], s5_c_re[layer], s5_c_im[layer],
                               s5_d[layer])
        glu = jax.nn.gelu(y_a, approximate=False) @ w_glu[layer]
        y_a = glu[..., :D_MODEL] * jax.nn.sigmoid(glu[..., D_MODEL:])
        y_b = ssd_bidirectional(z, xbc, dt_raw, conv_w[layer], conv_b[layer], ssd_dt_bias[layer],
                                ssd_a_log[layer], ssd_d[layer], ssd_norm_w[layer], w_ssd_out[layer])
        merged = jax.nn.sigmoid(gate_a) * y_a + jax.nn.sigmoid(gate_b) * y_b
        h = h + merged @ w_o[layer]
        h = h + peer_ffn(rmsnorm(h, ffn_norm_w[layer]), peer_w_q[layer], peer_sub_keys[layer],
                         peer_u[layer], peer_v[layer])
    return rmsnorm(h, final_norm_w).astype(x.dtype)
```

```python
import numpy as np
import ml_dtypes
from contextlib import ExitStack
import concourse.bass as bass
import concourse.mybir as mybir
from concourse.bass_utils import run_bass_kernel_spmd

F32 = mybir.dt.float32
BF16 = mybir.dt.bfloat16
I32 = mybir.dt.int32
U32 = mybir.dt.uint32
ALU = mybir.AluOpType
AF = mybir.ActivationFunctionType
AX = mybir.AxisListType

NCORES = 8
PADDED = ("w_in", "s5_par", "s5_B", "s5_C", "w_glu", "w_ssd_out", "w_o", "peer_w_q", "keysT_in", "peer_u", "peer_v")
SWEEP_ET = 128
PE_SKIP = False
CHAIN_OPT = True
ATTACH_ENG = ()
T = 4096
D = 1024
INW = 5152
C_U, C_Z, C_X, C_DT, C_GA, C_GB = 0, 512, 1536, 3072, 3104, 4128


class KB:
    def __init__(self, nc, es):
        self.nc = nc
        self.es = es
        self.es0 = es
        self.eng = {"pe": nc.tensor, "dve": nc.vector, "act": nc.scalar, "pool": nc.gpsimd, "sp": nc.sync}
        self.sem = {}
        self.cnt = {}
        for n in ["pe", "dve", "act", "pool"]:
            self.sem[n] = es.enter_context(nc.semaphore("s_" + n))
            self.cnt[n] = 0
        self.mult = {}
        self.waited = {}
        self.last_w = {}
        self.readers = {}
        self.ninst = 0
        self.rr = 0

    def dsem(self, key):
        k = "d_" + key
        if k not in self.sem:
            self.sem[k] = self.es0.enter_context(self.nc.semaphore(k))
            self.cnt[k] = 0
        return k

    def sb(self, name, shape, dt):
        return self.es.enter_context(self.nc.sbuf_tensor(name, list(shape), dt))

    def ps(self, name, shape, dt=F32):
        return self.es.enter_context(self.nc.psum_tensor(name, list(shape), dt))

    def _need(self, reads, writes):
        need = {}

        def add(sv):
            if sv is None:
                return
            s, v = sv
            if need.get(s, 0) < v:
                need[s] = v
        for r in reads:
            add(self.last_w.get(r))
        for w in writes:
            add(self.last_w.get(w))
            for sv in self.readers.get(w, ()):
                add(sv)
        return need

    def _emit_waits(self, e, need, attach=False):
        todo = []
        for s, v in need.items():
            if e == "pe" and s == "pe" and PE_SKIP:
                continue
            if self.waited.get((e, s), 0) >= v:
                continue
            todo.append((s, v))
            self.waited[(e, s)] = v
        keep = None
        if attach and todo and e in ATTACH_ENG:
            keep = todo.pop()
        for s, v in todo:
            mult = self.mult.get(s, 16 if s.startswith("d_") else 1)
            self.eng[e].wait_ge(self.sem[s], v * mult)
        if keep is not None:
            s, v = keep
            mult = self.mult.get(s, 16 if s.startswith("d_") else 1)
            return (self.sem[s], v * mult)
        return None

    def _record(self, s, v, reads, writes):
        for r in reads:
            self.readers.setdefault(r, []).append((s, v))
        for w in writes:
            self.last_w[w] = (s, v)
            self.readers[w] = []

    def op(self, e, fn, reads=(), writes=(), multi=False, noinc=False, noself=False):
        need = self._need(reads, writes)
        if noself:
            need.pop(e, None)
        att = self._emit_waits(e, need, attach=not multi)
        inst = fn(self.eng[e])
        if att is not None:
            inst.wait_op(att[0], att[1], "sem-ge")
        if noinc:
            self._record(e, self.cnt[e] + 1, reads, writes)
        else:
            self.cnt[e] += 1
            inst.then_inc(self.sem[e], 1)
            self._record(e, self.cnt[e], reads, writes)
        self.ninst += 1
        return inst

    def dma(self, q, key, out, in_, reads=(), writes=(), **kw):
        need = self._need(reads, writes)
        att = self._emit_waits(q, need, attach=True)
        k = self.dsem(key)
        inst = self.eng[q].dma_start(out=out, in_=in_, **kw)
        if att is not None:
            inst.wait_op(att[0], att[1], "sem-ge")
        self.cnt[k] += 1
        inst.then_inc(self.sem[k], 16)
        self._record(k, self.cnt[k], reads, writes)
        self.ninst += 1
        return inst

    def barrier(self):
        for e in ["pe", "dve", "act", "pool", "sp"]:
            for k, v in self.cnt.items():
                if v > 0 and self.waited.get((e, k), 0) < v:
                    mult = self.mult.get(k, 16 if k.startswith("d_") else 1)
                    self.eng[e].wait_ge(self.sem[k], v * mult)
                    self.waited[(e, k)] = v

    def scope(self):
        kb = self

        class _S:
            def __enter__(s2):
                s2.old = kb.es
                s2.st = ExitStack()
                s2.st.__enter__()
                kb.es_sems = s2.old
                kb.es = s2.st
                return s2

            def __exit__(s2, *a):
                kb.barrier()
                kb.es = s2.old
                return s2.st.__exit__(*a)
        return _S()

    def allgather(self, key, src_t, dst_t, reads, writes):
        need = self._need(reads, writes)
        self._emit_waits("pool", need)
        k = self.dsem(key)
        self.mult[k] = 1
        inst = self.nc.gpsimd.collective_compute("AllGather", ALU.bypass, replica_groups=[list(range(NCORES))],
                                                 ins=[src_t.ap().opt()], outs=[dst_t.ap().opt()])
        self.cnt[k] += 1
        inst.then_inc(self.sem[k])
        self._record(k, self.cnt[k], reads, writes)
        self.ninst += 1

    def wait_keys(self, e, keys):
        self._emit_waits(e, self._need(keys, keys))

    def mm(self, out, lhsT, rhs, start, stop, reads, writes, **kw):
        return self.op("pe", lambda e: e.matmul(out, lhsT, rhs, start=start, stop=stop, **kw), reads, writes,
                       noinc=(not stop) and CHAIN_OPT, noself=(not start) and CHAIN_OPT)

    def tr(self, out, in_, ident, reads, writes):
        return self.op("pe", lambda e: e.transpose(out, in_, ident), reads, writes)

    def evac_engine(self):
        self.rr += 1
        return "act" if self.rr % 2 else "dve"

    def copy(self, e, out, in_, reads, writes):
        if e == "act":
            return self.op("act", lambda g: g.activation(out, in_, AF.Copy), reads, writes)
        return self.op(e, lambda g: g.tensor_copy(out, in_), reads, writes)


class V:
    def __init__(self, ap, key):
        self.ap = ap
        self.key = key


class Tl:
    def __init__(self, t, key):
        self.t = t
        self.key = key

    def __getitem__(self, idx):
        return V(self.t[idx], self.key)

    def sub(self, idx, key):
        return V(self.t[idx], key)


def _ks(*vs):
    return [v.key for v in vs if isinstance(v, V)]


def _a(v):
    return v.ap if isinstance(v, V) else v


class OPS:
    def __init__(self, kb):
        self.kb = kb

    def tile(self, name, shape, dt):
        return Tl(self.kb.sb("t_" + name, shape, dt), name)

    def ptile(self, name, shape, dt=F32):
        return Tl(self.kb.ps("p_" + name, shape, dt), name)

    def tt(self, e, out, a, b, op):
        return self.kb.op(e, lambda g: g.tensor_tensor(out.ap, a.ap, b.ap, op), _ks(a, b), _ks(out))

    def ts(self, e, out, a, s1, s2, op0, op1=None):
        if op1 is None:
            return self.kb.op(e, lambda g: g.tensor_scalar(out.ap, a.ap, _a(s1), None, op0), _ks(a, s1), _ks(out))
        return self.kb.op(e, lambda g: g.tensor_scalar(out.ap, a.ap, _a(s1), _a(s2), op0, op1),
                          _ks(a, s1, s2), _ks(out))

    def stt(self, out, a, sc, b, op0, op1, e="dve"):
        return self.kb.op(e, lambda g: g.scalar_tensor_tensor(out.ap, a.ap, _a(sc), b.ap, op0, op1),
                          _ks(a, sc, b), _ks(out))

    def act(self, out, a, func, bias=None, scale=None, accum=None):
        kw = {}
        if bias is not None:
            kw["bias"] = _a(bias)
        if scale is not None:
            kw["scale"] = _a(scale)
        if accum is not None:
            kw["accum_out"] = accum.ap
        wr = _ks(out) + (_ks(accum) if accum is not None else [])
        return self.kb.op("act", lambda g: g.activation(out.ap, a.ap, func, **kw), _ks(a, bias, scale), wr,
                          multi=(accum is not None))

    def cp(self, e, out, a):
        if e == "act":
            return self.act(out, a, AF.Copy)
        return self.kb.op(e, lambda g: g.tensor_copy(out.ap, a.ap), _ks(a), _ks(out))

    def mm(self, out, lhsT, rhs, start=True, stop=True, noinc=None, noself=None, **kw):
        ni = ((not stop) if noinc is None else noinc) and CHAIN_OPT
        ns = ((not start) if noself is None else noself) and CHAIN_OPT
        return self.kb.op("pe", lambda g: g.matmul(out.ap, lhsT.ap, rhs.ap, start=start, stop=stop, **kw),
                          _ks(lhsT, rhs), _ks(out), noinc=ni, noself=ns)

    def tr(self, out, a, ident, noinc=False, noself=False):
        return self.kb.op("pe", lambda g: g.transpose(out.ap, a.ap, ident.ap), _ks(a, ident), _ks(out),
                          noinc=noinc and CHAIN_OPT, noself=noself and CHAIN_OPT)

    def dma(self, q, key, out, a, **kw):
        return self.kb.dma(q, key, out.ap, a.ap, reads=_ks(a), writes=_ks(out), **kw)

    def scan(self, out, d0, d1, init):
        return self.kb.op("dve", lambda g: g.tensor_tensor_scan(out.ap, d0.ap, d1.ap, _a(init), ALU.mult, ALU.add),
                          _ks(d0, d1, init), _ks(out))

    def memset(self, e, out, val):
        return self.kb.op(e, lambda g: g.memset(out.ap, val), [], _ks(out))

    def cmul(self, e, outr, outi, ar, ai, br, bi, t1, t2, neg_i=False):
        self.tt(e, t1, ar, br, ALU.mult)
        self.tt(e, t2, ai, bi, ALU.mult)
        self.tt(e, outr, t1, t2, ALU.subtract)
        self.tt(e, t1, ar, bi, ALU.mult)
        self.tt(e, t2, ai, br, ALU.mult)
        if neg_i:
            self.stt(outi, t1, -1.0, t2, ALU.mult, ALU.subtract)
        else:
            self.tt(e, outi, t1, t2, ALU.add)

    def cossin(self, e, ang, ni, s1, s2, outc, outs):
        self.cp(e, ni, ang)
        self.cp(e, s1, ni)
        self.tt(e, ang, ang, s1, ALU.subtract)
        self.act(s1, ang, AF.Sin, scale=float(np.pi))
        self.act(s2, ang, AF.Sin, scale=float(np.pi / 2))
        self.tt(e, s2, s2, s2, ALU.mult)
        self.ts(e, s2, s2, -2.0, 1.0, ALU.mult, ALU.add)
        self.tt(e, outs, s1, s2, ALU.mult)
        self.ts(e, outs, outs, 2.0, None, ALU.mult)
        self.tt(e, s1, s1, s1, ALU.mult)
        self.ts(e, outc, s1, -2.0, 1.0, ALU.mult, ALU.add)


def phase_s5_setup(kb, io, P):
    o = OPS(kb)
    W = 1024
    with kb.scope():
        t = [o.tile("s5t%d" % i, [128, W], F32) for i in range(14)]
        ni = o.tile("s5ni", [128, W], I32)
        ecol = o.tile("s5ecol", [128, 6], F32)
        msk = o.tile("s5msk", [128, 3, 128], F32)
        dsk = o.tile("s5dsk", [128, 32], F32)
        o.dma("sp", "s5ld", ecol[:], V(io["s5_ecol"][:, :], "in"))
        o.dma("sp", "s5ld", msk[:], V(io["s5_msk"][:, :, :], "in"))
        o.dma("sp", "s5ld", dsk[:], V(io["s5_dsk"][:, :], "in"))
        for d in range(2):
            for ti in (1, 2):
                P["s5tab"][d].append((o.tile("s5tab%d%d0" % (d, ti), [128, 2048], BF16),
                                      o.tile("s5tab%d%d1" % (d, ti), [128, 2048], BF16)))
        for d, hh in [(0, 0), (0, 1), (1, 0), (1, 1)]:
            cs = slice(hh * W, (hh + 1) * W)
            ar, ai, ld, ars, ais, rden, mag, ang, s1, s2, t11, t12, t13, t2 = [x[:] for x in t]
            nii = ni[:]
            o.dma("sp", "s5ld", ar, V(io["s5_par"][d, :, 0, cs], "in"))
            o.dma("sp", "s5ld", ai, V(io["s5_par"][d, :, 1, cs], "in"))
            o.dma("sp", "s5ld", ld, V(io["s5_par"][d, :, 2, cs], "in"))
            o.act(ld, ld, AF.Exp)
            o.tt("dve", ars, ar, ld, ALU.mult)
            o.tt("dve", ais, ai, ld, ALU.mult)
            o.ts("dve", ais, ais, float(1.0 / (2 * np.pi)), None, ALU.mult)
            o.tt("dve", rden, ar, ar, ALU.mult)
            o.tt("dve", t11, ai, ai, ALU.mult)
            o.tt("dve", rden, rden, t11, ALU.add)
            kb.op("dve", lambda g: g.reciprocal(rden.ap, rden.ap), [rden.key], [rden.key])
            o.act(mag, ars, AF.Exp)
            o.cp("dve", ang, ais)
            lr, li = t12, t13
            o.cossin("dve", ang, nii, s1, s2, lr, li)
            o.tt("dve", lr, lr, mag, ALU.mult)
            o.tt("dve", li, li, mag, ALU.mult)
            o.ts("dve", t11, lr, -1.0, None, ALU.add)
            o.tt("dve", s1, t11, ar, ALU.mult)
            o.tt("dve", s2, li, ai, ALU.mult)
            o.tt("dve", s1, s1, s2, ALU.add)
            fr = mag
            o.tt("dve", fr, s1, rden, ALU.mult)
            o.tt("dve", s1, li, ar, ALU.mult)
            o.tt("dve", s2, t11, ai, ALU.mult)
            o.tt("dve", s1, s1, s2, ALU.subtract)
            fi = ang
            o.tt("dve", fi, s1, rden, ALU.mult)
            o.dma("sp", "s5ld", ar, V(io["s5_B"][d, :, 0, cs], "in"))
            o.dma("sp", "s5ld", ai, V(io["s5_B"][d, :, 1, cs], "in"))
            Btr, Bti = rden, t11
            o.cmul("dve", Btr, Bti, fr, fi, ar, ai, s1, s2)
            Cr, Ci = ar, ai
            o.dma("sp", "s5ld", Cr, V(io["s5_C"][d, :, 0, cs], "in"))
            o.dma("sp", "s5ld", Ci, V(io["s5_C"][d, :, 1, cs], "in"))
            for ti in range(3):
                ecolv = ecol[:, d * 3 + ti: d * 3 + ti + 1]
                o.act(mag, ars, AF.Exp, scale=ecolv)
                o.ts("dve", ang, ais, ecolv, None, ALU.mult)
                o.cossin("dve", ang, nii, s1, s2, lr, li)
                o.tt("dve", lr, lr, mag, ALU.mult)
                o.tt("dve", li, li, mag, ALU.mult)
                tr_, ti_ = P["s5tab"][d][ti]
                if ti == 0:
                    o.cmul("dve", tr_[:, cs], ti_[:, cs], lr, li, Btr, Bti, s1, s2)
                else:
                    o.cmul("dve", tr_[:, cs], ti_[:, cs], lr, li, Cr, Ci, s1, s2, neg_i=True)
        ident = P["identb"]
        ptr = [o.ptile("s5ptr%d" % i, [128, 128], BF16) for i in range(4)]
        pmm = [o.ptile("s5pmm%d" % i, [128, 128], F32) for i in range(4)]
        trs = [o.tile("s5trs%d" % i, [128, 128], BF16) for i in range(8)]
        macc = o.tile("s5macc", [128, 128], F32)
        macc2 = o.tile("s5macc2", [128, 128], F32)
        k = 0
        for gp in range(16):
            sl = slice(gp * 128, (gp + 1) * 128)
            TT = {}
            for d in range(2):
                for ti in range(3):
                    for c in range(2):
                        src = P["s5tab"][d][ti][c][:, sl]
                        pt = ptr[k % 4]
                        o.tr(pt[:], src, ident[:])
                        if ti == 1:
                            dst = P["s5wout"][d][c][:, gp, :]
                        else:
                            dst = trs[(d * 2 + (ti // 2)) * 2 + c][:]
                        o.cp(kb.evac_engine(), dst, pt[:])
                        TT[(d, ti, c)] = dst
                        k += 1
            for gpar in range(2):
                g = gp * 2 + gpar
                pr = slice(gpar * 64, gpar * 64 + 64)
                pms = []
                for d in range(2):
                    pm = pmm[(g * 2 + d) % 4]
                    for c in range(2):
                        lt = TT[(d, 0, c)]
                        rt = TT[(d, 2, c)]
                        o.mm(pm[:], V(lt.ap[pr, :], lt.key), V(rt.ap[pr, :], rt.key), start=(c == 0), stop=(c == 1))
                    pms.append(pm)
                o.tt("dve", macc[:], pms[0][:], msk[:, 0, :], ALU.mult)
                o.tt("dve", macc2[:], pms[1][:], msk[:, 1, :], ALU.mult)
                o.tt("dve", macc[:], macc[:], macc2[:], ALU.add)
                o.stt(P["s5mintra"][:, g, :], msk[:, 2, :], dsk[:, g:g + 1], macc[:], ALU.mult, ALU.add)
        for d in range(2):
            del P["s5tab"][d][1:]


def s5_shuffle_u(kb, io, P):
    U = P["U"]
    qs = ["sp", "act", "pool"]
    k = 0
    for g in range(32):
        for s_ in range(8):
            kb.dma(qs[k % 3], "ushuf", U.t[16 * s_:16 * s_ + 16, g, :], io["uT2d"][16 * g:16 * g + 16, s_, :],
                   reads=["uT2d"], writes=["U"])
            k += 1


def s5_main(kb, io, P):
    o = OPS(kb)
    U = P["U"]
    with kb.scope():
        NB = 2
        Wd = NB * 512
        Sp = [[o.tile("s5sp%d%d" % (d, c), [128, 16, 513], BF16) for c in range(2)] for d in range(2)]
        par2 = o.tile("s5par2", [128, 3, 16], F32)
        r8 = [o.tile("s5r8_%d" % d, [128, 16], F32) for d in range(2)]
        fr8 = [o.tile("s5fr8_%d" % d, [128, 16], F32) for d in range(2)]
        cs8 = [[o.tile("s5cs8_%d%d" % (d, c), [128, 16], F32) for c in range(2)] for d in range(2)]
        W0 = [[o.tile("s5w0_%d%d" % (d, c), [128, 16], F32) for c in range(2)] for d in range(2)]
        sm = [o.tile("s5sm%d" % i, [128, 16], F32) for i in range(4)]
        smi = o.tile("s5smi", [128, 16], I32)
        miota = o.tile("s5miota", [128, 512], F32)
        o.dma("sp", "s5ld2", miota[:], V(io["miota"][:, :], "in"))
        for d in range(2):
            o.dma("sp", "s5ld2", par2[:], V(io["s5_par2"][d], "in"))
            o.act(sm[0][:], par2[:, 2, :], AF.Exp)
            o.tt("dve", sm[1][:], par2[:, 0, :], sm[0][:], ALU.mult)
            o.act(r8[d][:], sm[1][:], AF.Exp, scale=8.0)
            o.tt("dve", sm[1][:], par2[:, 1, :], sm[0][:], ALU.mult)
            o.ts("dve", sm[1][:], sm[1][:], float(8.0 / (2 * np.pi)), None, ALU.mult)
            o.cp("dve", smi[:], sm[1][:])
            o.cp("dve", sm[2][:], smi[:])
            o.tt("dve", fr8[d][:], sm[1][:], sm[2][:], ALU.subtract)
            o.cp("dve", sm[1][:], fr8[d][:])
            o.cossin("dve", sm[1][:], smi[:], sm[2][:], sm[3][:], cs8[d][0][:], cs8[d][1][:])
        Er = o.tile("m_s5Er", [128, NB, 512], F32)
        Ei = o.tile("m_s5Ei", [128, NB, 512], F32)
        ang = o.tile("m_s5ang", [128, NB, 512], F32)
        t1 = o.tile("m_s5t1", [128, NB, 512], F32)
        t2 = o.tile("m_s5t2", [128, NB, 512], F32)
        cosT = o.tile("m_s5cos", [128, NB, 512], F32)
        sinT = o.tile("m_s5sin", [128, NB, 512], F32)
        Wr = o.tile("m_s5Wr", [128, NB, 512], F32)
        Wi = o.tile("m_s5Wi", [128, NB, 512], F32)
        nii = o.tile("m_s5nii", [128, NB, 512], I32)
        pE = [o.ptile("s5pE%d" % i, [128, 512], F32) for i in range(4)]
        sendb = o.tile("s5send", [128, 64], F32)
        recvb = o.tile("s5recv", [128, 8, 64], F32)
        sin_ = o.tile("s5sin_in", [128, 64], F32)
        sel = o.tile("s5sel", [128, 16], F32)
        o.dma("sp", "s5ld2", sel[:], V(io["sel"][:, :], "in"))

        def fl(tl):
            return V(tl.t[:].rearrange("p a b -> p (a b)"), tl.key)

        def scan_pass(use_init):
            for d in range(2):
                for c in range(2):
                    col = 0 if d == 0 else 512
                    if use_init:
                        o.cp("dve", Sp[d][c][:, :, col], sin_[:, d * 32 + c * 16: d * 32 + c * 16 + 16])
                    else:
                        o.memset("dve", Sp[d][c][:, :, col], 0.0)
                if use_init:
                    o.cmul("dve", W0[d][0][:], W0[d][1][:], sin_[:, d * 32:d * 32 + 16], sin_[:, d * 32 + 16:d * 32 + 32],
                           cs8[d][0][:], cs8[d][1][:], sm[0][:], sm[1][:])
                for blk in range(16 // NB):
                    gp0 = blk * NB
                    for j in range(NB):
                        gp = gp0 + j
                        for c in range(2):
                            pt = pE[(j * 2 + c) % 4]
                            for gpar in range(2):
                                g = gp * 2 + gpar
                                o.mm(V(pt.t[gpar * 64:(gpar + 1) * 64, :], pt.key),
                                     P["s5tab"][d][0][c][:, g * 64:(g + 1) * 64], U[:, g, :])
                            dstt = Er if c == 0 else Ei
                            dst = dstt[:, j, :] if d == 0 else dstt[:, j, ::-1]
                            o.cp(kb.evac_engine(), dst, pt[:])
                        o.ts("dve", ang[:, j, :], miota[:], fr8[d][:, gp:gp + 1], None, ALU.mult)
                    o.cossin("dve", fl(ang), fl(nii), fl(t1), fl(t2), fl(cosT), fl(sinT))
                    o.ts("dve", fl(t2), fl(sinT), -1.0, None, ALU.mult)
                    o.cmul("dve", fl(Wr), fl(Wi), fl(Er), fl(Ei), fl(cosT), fl(t2), fl(t1), fl(ang))
                    for j in range(NB):
                        gp = gp0 + j
                        for c, (src, dstw) in enumerate([(Wr, Er), (Wi, Ei)]):
                            init = W0[d][c][:, gp:gp + 1] if use_init else 0.0
                            o.scan(dstw[:, j, :], V(r8[d].t[:, gp:gp + 1].to_broadcast([128, 512]), r8[d].key),
                                   src[:, j, :], init)
                    if d == 0:
                        outr = Sp[0][0][:, gp0:gp0 + NB, 1:513]
                        outi = Sp[0][1][:, gp0:gp0 + NB, 1:513]
                    else:
                        outr = Sp[1][0][:, gp0:gp0 + NB, 511::-1]
                        outi = Sp[1][1][:, gp0:gp0 + NB, 511::-1]
                    o.cmul("dve", outr, outi, Er[:], Ei[:], cosT[:], sinT[:], t1[:], t2[:])

        scan_pass(False)
        for d in range(2):
            for c in range(2):
                col = 512 if d == 0 else 0
                o.cp("dve", sendb[:, d * 32 + c * 16:d * 32 + c * 16 + 16], Sp[d][c][:, :, col])
        o.dma("pool", "s5xs", V(P["cc_s5_src"][:, :], "cc_s5_src"), sendb[:])
        kb.allgather("s5cc", P["cc_s5_src_t"], P["cc_s5_dst_t"], reads=["cc_s5_src"], writes=["cc_s5_dst"])
        o.dma("pool", "s5xr", recvb[:], V(P["cc_s5_dst"].rearrange("(r p) c -> p r c", p=128), "cc_s5_dst"))
        for d in range(2):
            sl = slice(d * 32, d * 32 + 32)
            o.ts("dve", sin_[:, sl], recvb[:, 0, sl], sel[:, d * 8:d * 8 + 1], None, ALU.mult)
            for r in range(1, 8):
                o.stt(sin_[:, sl], recvb[:, r, sl], sel[:, d * 8 + r:d * 8 + r + 1], sin_[:, sl], ALU.mult, ALU.add)
        scan_pass(True)
        pY = [o.ptile("s5pY%d" % i, [128, 512], F32) for i in range(2)]
        Yg = [o.tile("s5Yg%d" % i, [128, 512], BF16) for i in range(2)]
        qs = ["sp", "act", "pool"]
        kq = 0
        for g in range(32):
            gp, gpar = g // 2, g % 2
            pr = slice(gpar * 64, gpar * 64 + 64)
            py = pY[g % 2]
            o.mm(py[:], P["s5mintra"][:, g, :], U[:, g, :], start=True, stop=False)
            k = 0
            for d in range(2):
                for c in range(2):
                    cols = slice(0, 512) if d == 0 else slice(1, 513)
                    wo = P["s5wout"][d][c]
                    o.mm(py[:], V(wo.t[pr, gp, :], wo.key), V(Sp[d][c].t[pr, gp, cols], Sp[d][c].key),
                         start=False, stop=(k == 3))
                    k += 1
            yg = Yg[g % 2]
            o.act(yg[:], py[:], AF.Gelu)
            for tau in range(8):
                o.dma(qs[kq % 3], "s5yst", V(io["ys5P"][16 * g:16 * g + 16, tau, :], "ys5P"),
                      V(yg.t[16 * tau:16 * tau + 16, :], yg.key))
                kq += 1


def ssd_phase(kb, io, P):
    o = OPS(kb)
    dt_raw = Tl(kb.dt_raw, "dt_raw")
    NEG = -30000.0
    with kb.scope():
        cw = o.tile("cw", [128, 12, 5], F32)
        cb = o.tile("cb", [128, 12], F32)
        cst = o.tile("ssd_cst", [128, 32 + 32 + 16], F32)
        nwbc = o.tile("ssd_nw", [128, 1024], F32)
        mats = o.tile("ssd_mats", [128, 5, 128], F32)
        identf = Tl(kb.ident, "ident")
        o.dma("sp", "ssdld", cw[:], V(io["conv_w_pc"][:, :, :], "in"))
        o.dma("sp", "ssdld", cb[:], V(io["conv_b_pc"][:, :], "in"))
        o.dma("sp", "ssdld", cst[:], V(io["ssd_cst"][:, :], "in"))
        o.dma("sp", "ssdld", nwbc[:], V(io["ssd_nw_bc"][:, :], "in"))
        o.dma("sp", "ssdld", mats[:], V(io["ssd_mats"][:, :, :], "in"))
        nega = o.tile("ssd_nega", [128, 32], F32)
        o.act(nega[:], cst[:, 32:64], AF.Exp)
        o.ts("dve", nega[:], nega[:], -1.0, None, ALU.mult)
        BT = [o.tile("ssd_BT%d" % j, [128, T], BF16) for j in range(2)]
        CT = [o.tile("ssd_CT%d" % j, [128, T], BF16) for j in range(2)]
        prev = [o.tile("ssd_prev%d" % d, [128, 32, 512], BF16) for d in range(2)]
        dtt = o.tile("ssd_dt", [128, 32, 32], F32)
        adt = o.tile("ssd_adt", [128, 32, 32], F32)
        cs = o.tile("ssd_cs", [128, 32, 32], F32)
        tot = o.tile("ssd_tot", [128, 32, 32], F32)
        ecs = o.tile("ssd_ecs", [128, 32, 32], F32)
        dst8 = o.tile("ssd_dstate", [128, 32, 32], F32)
        dec = o.tile("ssd_dec", [128, 32, 32], F32)
        pbf = o.ptile("ssd_pbf", [128, 8, 128], BF16)
        pD = [o.ptile("ssd_pD%d" % i, [128, 512], F32) for i in range(2)]
        pG = o.ptile("ssd_pG", [128, 512], F32)
        pY = o.ptile("ssd_pY", [128, 1024], F32)
        pO = o.ptile("ssd_pO", [128, 1024], F32)
        identb = P["identb"]

        with kb.scope():
            xin = [o.tile("ssd_xin%d" % i, [128, T + 4], BF16) for i in range(2)]
            acc = o.tile("ssd_acc", [128, 2048], F32)
            xc = [o.tile("ssd_xc%d" % i, [128, T], BF16) for i in range(2)]
            stg = [o.tile("ssd_stg%d" % i, [128, 128], BF16) for i in range(4)]
            k = 0
            for ct in range(12):
                xi = xin[ct % 2]
                o.dma("sp", "ssd_xin%d" % (ct % 2), xi[:], V(io["xbcT"][ct * 128:(ct + 1) * 128, :], "xbcT"))
                if ct < 8:
                    dst = xc[ct % 2]
                elif ct < 10:
                    dst = BT[ct - 8]
                else:
                    dst = CT[ct - 10]
                for hf in range(2):
                    c0 = hf * 2048
                    o.ts("dve", acc[:], xi[:, c0:c0 + 2048], cw[:, ct, 0:1], None, ALU.mult)
                    for kk in range(1, 5):
                        o.stt(acc[:], xi[:, c0 + kk:c0 + kk + 2048], cw[:, ct, kk:kk + 1], acc[:], ALU.mult, ALU.add)
                    o.act(dst[:, c0:c0 + 2048], acc[:], AF.Silu, bias=cb[:, ct:ct + 1])
                if ct < 10:
                    for c in range(32):
                        o.tr(pbf[:, k % 8, :], dst[:, c * 128:(c + 1) * 128], identb[:])
                        st = stg[k % 4]
                        o.cp(kb.evac_engine(), st[:], pbf[:, k % 8, :])
                        if ct < 8:
                            o.dma("sp", "ssd_st", V(io["x_tm"][c * 128:(c + 1) * 128, ct * 128:(ct + 1) * 128], "x_tm"), st[:])
                        else:
                            o.dma("sp", "ssd_st", V(io["B_tm"][c * 128:(c + 1) * 128, (ct - 8) * 128:(ct - 7) * 128], "B_tm"), st[:])
                        k += 1
        def bc32(v):
            return V(v.ap.unsqueeze(1).to_broadcast([128, 32, 32]), v.key)
        o.tt("dve", dtt[:], dt_raw[:], bc32(cst[:, 0:32]), ALU.add)
        o.act(dtt[:], dtt[:], AF.Exp)
        o.act(dtt[:], dtt[:], AF.Ln, bias=1.0)
        o.tt("dve", adt[:], dtt[:], bc32(nega[:]), ALU.mult)
        for d in range(2):
            rhs = adt[:, :, d * 16:(d + 1) * 16]
            o.mm(V(pD[0].t[:, :].rearrange("p (c h) -> p c h", h=16), pD[0].key), mats[:, d, :], rhs)
            o.cp("dve", cs[:, :, d * 16:(d + 1) * 16], V(pD[0].t[:, :].rearrange("p (c h) -> p c h", h=16), pD[0].key))
            o.mm(V(pD[1].t[:, :].rearrange("p (c h) -> p c h", h=16), pD[1].key), mats[:, 4, :], rhs)
            o.cp("dve", tot[:, :, d * 16:(d + 1) * 16], V(pD[1].t[:, :].rearrange("p (c h) -> p c h", h=16), pD[1].key))
        o.act(ecs[:], cs[:], AF.Exp)
        o.act(dec[:], tot[:], AF.Exp)
        o.tt("dve", dst8[:], tot[:], cs[:], ALU.subtract)
        o.act(dst8[:], dst8[:], AF.Exp)

        R = [o.tile("ssd_R%d" % d, [128, 2, 256], F32) for d in range(2)]
        xt_ = [o.tile("ssd_xt%d" % i, [128, 1024], BF16) for i in range(2)]
        bt_ = [o.tile("ssd_bt%d" % i, [128, 256], BF16) for i in range(2)]
        xdd = [o.tile("ssd_xdd%d" % i, [128, 1024], BF16) for i in range(2)]
        sw = [o.tile("ssd_sw%d" % d, [128, 32, 16], F32) for d in range(2)]
        pS = V(pO.t[:, 0:512].rearrange("p (a b) -> p a b", a=2), pO.key)

        def hb(v, h0, nh, w):
            return V(v.ap.unsqueeze(2).to_broadcast([128, nh, w]), v.key)

        def v3(tl, nh, w):
            return V(tl.t[:, :].rearrange("p (h w) -> p h w", w=w), tl.key)

        for d in range(2):
            o.memset("dve", R[d][:], 0.0)
            order = range(32) if d == 0 else range(31, -1, -1)
            for c in order:
                b = c % 2
                o.dma("sp", "ssd_xl%d" % b, xt_[b][:], V(io["x_tm"][c * 128:(c + 1) * 128, :], "x_tm"))
                o.dma("sp", "ssd_bl%d" % b, bt_[b][:], V(io["B_tm"][c * 128:(c + 1) * 128, :], "B_tm"))
                o.tt("dve", sw[d][:, c, :], dst8[:, c, d * 16:(d + 1) * 16], dtt[:, c, d * 16:(d + 1) * 16], ALU.mult)
                o.tt("dve", v3(xdd[b], 16, 64), v3(xt_[b], 16, 64), hb(sw[d][:, c, :], 0, 16, 64), ALU.mult)
                for g in range(4):
                    o.mm(V(pO.t[(g % 2) * 64:(g % 2) * 64 + 64, (g // 2) * 256:(g // 2) * 256 + 256], pO.key),
                         bt_[b][:, g * 64:(g + 1) * 64], xdd[b][:, g * 256:(g + 1) * 256])
                o.cp("act", V(prev[d].t[:, c, :].rearrange("p (a b) -> p a b", a=2), prev[d].key), R[d][:])
                for g2 in range(2):
                    for gpar in range(2):
                        g = g2 * 2 + gpar
                        pr = slice(gpar * 64, gpar * 64 + 64)
                        dv = dec[:, c, d * 16 + g * 4:d * 16 + g * 4 + 4]
                        o.tt("dve", V(R[d].t[pr, g2, :].rearrange("p (h w) -> p h w", w=64), R[d].key),
                             V(R[d].t[pr, g2, :].rearrange("p (h w) -> p h w", w=64), R[d].key),
                             V(dv.ap[pr].unsqueeze(2).to_broadcast([64, 4, 64]), dv.key), ALU.mult)
                o.tt("dve", R[d][:], R[d][:], pS, ALU.add)
        P["ssd_state"] = dict(R=R, prev=prev)
        sendb = o.tile("ssd_send", [128, 1024], F32)
        for d in range(2):
            o.cp("dve", sendb[:, d * 512:(d + 1) * 512], V(R[d].t[:].rearrange("p a b -> p (a b)"), R[d].key))
        o.dma("pool", "ssdxs", V(P["cc_ssd_src"][:, :], "cc_ssd_src"), sendb[:])
        kb.allgather("ssdcc", P["cc_ssd_src_t"], P["cc_ssd_dst_t"], reads=["cc_ssd_src"], writes=["cc_ssd_dst"])
        sel = o.tile("ssd_sel", [128, 16], F32)
        o.dma("sp", "ssdld", sel[:], V(io["sel"][:, :], "in"))
        sin_ = o.tile("ssd_sin", [128, 1024], F32)
        with kb.scope():
            recvb = o.tile("ssd_recv", [128, 8, 1024], F32)
            o.dma("pool", "ssdxr", recvb[:], V(P["cc_ssd_dst"].rearrange("(r p) c -> p r c", p=128), "cc_ssd_dst"))
            for d in range(2):
                sl = slice(d * 512, d * 512 + 512)
                o.ts("dve", sin_[:, sl], recvb[:, 0, sl], sel[:, d * 8:d * 8 + 1], None, ALU.mult)
                for r in range(1, 8):
                    o.stt(sin_[:, sl], recvb[:, r, sl], sel[:, d * 8 + r:d * 8 + r + 1], sin_[:, sl], ALU.mult, ALU.add)
        cum = o.tile("ssd_cum", [128, 16], F32)
        ecum = o.tile("ssd_ecum", [128, 16], F32)
        corr = o.tile("ssd_corr", [128, 2, 256], F32)
        for d in range(2):
            o.memset("dve", cum[:], 0.0)
            order = range(32) if d == 0 else range(31, -1, -1)
            for c in order:
                o.act(ecum[:], cum[:], AF.Exp)
                for g2 in range(2):
                    for gpar in range(2):
                        g = g2 * 2 + gpar
                        pr = slice(gpar * 64, gpar * 64 + 64)
                        ev = ecum[:, g * 4:g * 4 + 4]
                        o.tt("dve", V(corr.t[pr, g2, :].rearrange("p (h w) -> p h w", w=64), corr.key),
                             V(sin_.t[pr, d * 512 + g2 * 256:d * 512 + g2 * 256 + 256].rearrange("p (h w) -> p h w", w=64), sin_.key),
                             V(ev.ap[pr].unsqueeze(2).to_broadcast([64, 4, 64]), ev.key), ALU.mult)
                pv = V(prev[d].t[:, c, :].rearrange("p (a b) -> p a b", a=2), prev[d].key)
                o.tt("dve", pv, pv, corr[:], ALU.add)
                o.tt("dve", cum[:], cum[:], tot[:, c, d * 16:(d + 1) * 16], ALU.add)
        Ms = o.tile("ssd_M", [128, 2, 16, 128], BF16)
        Gs = o.tile("ssd_G", [128, 4, 128], BF16)
        rhsA = o.tile("ssd_rhsA", [128, 4, 128], F32)
        Lt = [o.tile("ssd_L%d" % i, [128, 4, 128], BF16) for i in range(2)]
        xdt = [o.tile("ssd_xdt%d" % i, [128, 1024], BF16) for i in range(2)]
        zs_ = [o.tile("ssd_zs%d" % i, [128, 1024], BF16) for i in range(2)]
        yf = o.tile("ssd_yf", [128, 1024], F32)
        yt = o.tile("ssd_yt", [128, 1024], F32)
        ysq = yt
        ssq = o.tile("ssd_ssq", [128, 4], F32)
        ynb = o.tile("ssd_ynb", [128, 1024], BF16)
        ynT = [o.tile("ssd_ynT%d" % i, [128, 8, 128], BF16) for i in range(2)]
        negcs = o.tile("ssd_negcs", [128, 32, 32], F32)
        o.ts("dve", negcs[:], cs[:], -1.0, None, ALU.mult)
        negm = [o.tile("ssd_negm%d" % d, [128, 4, 128], F32) for d in range(2)]
        for d in range(2):
            o.cp("dve", negm[d][:], V(mats.t[:, 2 + d, :].unsqueeze(1).to_broadcast([128, 4, 128]), mats.key))
        identf3 = V(kb.ident[:, :].unsqueeze(1).to_broadcast([128, 4, 128]), "ident")
        dsk = cst[:, 64:80]
        for c in range(32):
            b = c % 2
            tk = slice(c * 128, (c + 1) * 128)
            o.dma("sp", "ssd_xl%d" % b, xt_[b][:], V(io["x_tm"][tk, :], "x_tm"))
            o.dma("sp", "ssd_zl%d" % b, zs_[b][:], V(io["zs"][tk, :], "zs"))
            for g in range(4):
                pr = slice((g % 2) * 64, (g % 2) * 64 + 64)
                bt, ct_ = BT[g // 2], CT[g // 2]
                o.mm(pG[:, g * 128:(g + 1) * 128], V(bt.t[pr, tk], bt.key), V(ct_.t[pr, tk], ct_.key))
            o.cp("act", V(Gs.t[:].rearrange("p a b -> p (a b)"), Gs.key), pG[:])
            k = 0
            for d in range(2):
                for hg in range(4):
                    pd = pD[k % 2]
                    k += 1
                    csv = cs[:, c, d * 16 + hg * 4:d * 16 + hg * 4 + 4]
                    o.tt("dve", rhsA[:], identf3, V(csv.ap.unsqueeze(2).to_broadcast([128, 4, 128]), csv.key), ALU.mult)
                    o.mm(pd[:], mats[:, 4, :], V(rhsA.t[:].rearrange("p a b -> p (a b)"), rhsA.key), start=True, stop=False)
                    o.mm(pd[:], identf[:], V(negm[d].t[:].rearrange("p a b -> p (a b)"), negm[d].key), start=False, stop=True)
                    lt = Lt[k % 2]
                    for hh in range(4):
                        h = hg * 4 + hh
                        o.act(lt[:, hh, :], pd[:, hh * 128:(hh + 1) * 128], AF.Exp,
                              bias=negcs[:, c, d * 16 + h:d * 16 + h + 1])
                    o.tt("dve", Ms[:, d, hg * 4:(hg + 1) * 4, :], lt[:],
                         V(Gs.t[:, hg, :].unsqueeze(1).to_broadcast([128, 4, 128]), Gs.key), ALU.mult)
                o.tt("dve", v3(xdt[d], 16, 64), v3(xt_[b], 16, 64), hb(dtt[:, c, d * 16:(d + 1) * 16], 0, 16, 64), ALU.mult)
            for h in range(16):
                for d in range(2):
                    o.mm(pY[:, h * 64:(h + 1) * 64], Ms[:, d, h, :], xdt[d][:, h * 64:(h + 1) * 64],
                         start=(d == 0), stop=(d == 1))
            o.cp("dve", yf[:], pY[:])
            for d in range(2):
                for g in range(4):
                    pr = slice((g % 2) * 64, (g % 2) * 64 + 64)
                    ct_ = CT[g // 2]
                    o.mm(pO[:, g * 256:(g + 1) * 256], V(ct_.t[pr, tk], ct_.key),
                         V(prev[d].t[pr, c, (g // 2) * 256:(g // 2) * 256 + 256], prev[d].key))
                o.tt("dve", v3(yt, 16, 64), V(pO.t[:, :].rearrange("p (h w) -> p h w", w=64), pO.key),
                     hb(ecs[:, c, d * 16:(d + 1) * 16], 0, 16, 64), ALU.mult)
                o.tt("dve", yf[:], yf[:], yt[:], ALU.add)
            o.tt("dve", v3(yt, 16, 64), v3(xt_[b], 16, 64), hb(dsk, 0, 16, 64), ALU.mult)
            o.tt("dve", yf[:], yf[:], yt[:], ALU.add)
            o.tt("dve", yf[:], yf[:], zs_[b][:], ALU.mult)
            o.tt("dve", ysq[:], yf[:], yf[:], ALU.mult)
            kb.op("dve", lambda g_: g_.tensor_reduce(ssq.t[:, :], ysq.t[:, :].rearrange("p (g w) -> p g w", w=256),
                                                      AX.X, ALU.add), [ysq.key], [ssq.key])
            o.ts("dve", ssq[:], ssq[:], 1.0 / 256, 1e-6, ALU.mult, ALU.add)
            o.act(ssq[:], ssq[:], AF.Sqrt)
            kb.op("dve", lambda g_: g_.reciprocal(ssq.t[:, :], ssq.t[:, :]), [ssq.key], [ssq.key])
            o.tt("dve", v3(yf, 4, 256), v3(yf, 4, 256), hb(ssq[:], 0, 4, 256), ALU.mult)
            o.tt("dve", ynb[:], yf[:], nwbc[:], ALU.mult)
            for j in range(8):
                o.tr(pbf[:, j, :], ynb[:, j * 128:(j + 1) * 128], identb[:])
            o.cp("act", ynT[b][:], pbf[:])
            o.dma("sp", "ssd_yst", V(io["ynT"].rearrange("(j p) t -> p j t", p=128)[:, :, tk], "ynT"), ynT[b][:])


def load_w_bf16(kb, o, name, dram, rows, cols, scale_col=None):
    nt = rows // 128
    w = o.tile(name, [128, nt, cols], BF16)
    CH = 1024
    with kb.scope():
        stg = [o.tile(name + "_stg%d" % i, [128, CH], F32) for i in range(2)]
        k = 0
        for t in range(nt):
            for c0 in range(0, cols, CH):
                cw_ = min(CH, cols - c0)
                st = stg[k % 2]
                o.dma("sp", name + "_ld%d" % (k % 2), st[:, 0:cw_], V(dram[t * 128:(t + 1) * 128, c0:c0 + cw_], "in"))
                o.cp(["dve", "pool"][k % 2], w.sub((slice(None), t, slice(c0, c0 + cw_)), name + "_%d" % t), st[:, 0:cw_])
                k += 1
    return w


def post_phase(kb, io, P):
    o = OPS(kb)
    with kb.scope():
        wglu = load_w_bf16(kb, o, "wglu", io["w_glu"], 512, 2048)
        wso = load_w_bf16(kb, o, "wso", io["w_ssd_out"], 1024, 1024)
        wo = load_w_bf16(kb, o, "wo", io["w_o"], 1024, 1024)
        wgk = ["wglu_%d" % t for t in range(4)]
        wsk = ["wso_%d" % t for t in range(8)]
        wok = ["wo_%d" % t for t in range(8)]
        fnw = o.tile("post_fnw", [128, 1024], F32)
        o.dma("sp", "postld", fnw[:], V(io["ffn_nw_bc"][:, :], "in"))
        identf = Tl(kb.ident, "ident")
        ys = [o.tile("post_ys%d" % i, [128, 4, 8, 64], BF16) for i in range(2)]
        yn = [o.tile("post_yn%d" % i, [128, 8, 512], BF16) for i in range(2)]
        ga = [o.tile("post_ga%d" % i, [128, 512], BF16) for i in range(2)]
        gb = [o.tile("post_gb%d" % i, [128, 512], BF16) for i in range(2)]
        sg = [o.tile("post_sg%d" % i, [128, 512], F32) for i in range(2)]
        ya = [o.tile("post_ya%d" % i, [128, 512], F32) for i in range(2)]
        m1 = [o.tile("post_m1%d" % i, [128, 512], F32) for i in range(2)]
        m2 = [o.tile("post_m2%d" % i, [128, 512], F32) for i in range(2)]
        mT = o.tile("post_mT", [128, 8, 512], BF16)
        xt = o.tile("post_xt", [128, 4, 1024], F32)
        hsq = o.tile("post_hsq", [128, 1024], BF16)
        hss = o.tile("post_hss", [128, 4], F32)
        hn = o.tile("post_hn", [128, 1024], F32)
        hnT = o.tile("post_hnT", [128, 8, 512], BF16)
        pp = [o.ptile("post_p%d" % i, [128, 512], F32) for i in range(8)]
        pi = [0]

        def nps():
            pi[0] = (pi[0] + 1) % 8
            return pp[pi[0]]

        for tt in range(8):
            b = tt % 2
            tk = slice(tt * 512, (tt + 1) * 512)
            for ct in range(4):
                o.dma("sp", "post_ysl%d" % b, ys[b][:, ct, :, :],
                      V(io["ys5P"][ct * 128:(ct + 1) * 128, :, tt * 64:(tt + 1) * 64], "ys5P"))
            o.dma("sp", "post_ynl%d" % b, yn[b][:],
                  V(io["ynT"].rearrange("(j p) t -> p j t", p=128)[:, :, tk], "ynT"))
            o.dma("sp", "post_xl", xt[:], V(io["x_ext"][2 + tt * 512:2 + (tt + 1) * 512, :].rearrange("(s p) d -> p s d", p=128), "in"))
            for j in range(8):
                jb = j % 2
                o.dma("sp", "post_gal%d" % jb, ga[jb][:], V(io["gT"][j * 128:(j + 1) * 128, tk], "gT"))
                o.dma("sp", "post_gbl%d" % jb, gb[jb][:], V(io["gT"][1024 + j * 128:1024 + (j + 1) * 128, tk], "gT"))
                p1, p2, p3 = nps(), nps(), nps()
                for ct in range(4):
                    rhs = V(ys[b].t[:, ct, :, :].rearrange("p a b -> p (a b)"), ys[b].key)
                    o.mm(p1[:], V(wglu.t[:, ct, j * 128:(j + 1) * 128], wgk[ct]), rhs, start=(ct == 0), stop=(ct == 3))
                for ct in range(4):
                    rhs = V(ys[b].t[:, ct, :, :].rearrange("p a b -> p (a b)"), ys[b].key)
                    o.mm(p2[:], V(wglu.t[:, ct, 1024 + j * 128:1024 + (j + 1) * 128], wgk[ct]), rhs,
                         start=(ct == 0), stop=(ct == 3))
                for jt in range(8):
                    o.mm(p3[:], V(wso.t[:, jt, j * 128:(j + 1) * 128], wsk[jt]), yn[b][:, jt, :],
                         start=(jt == 0), stop=(jt == 7))
                o.act(sg[jb][:], p2[:], AF.Sigmoid)
                o.tt("dve", ya[jb][:], p1[:], sg[jb][:], ALU.mult)
                ya_nat = V(ya[jb].t[:, :].rearrange("p (t n) -> p n t", t=8), ya[jb].key)
                o.tt("dve", V(m1[jb].t[:, :].rearrange("p (n t) -> p n t", t=8), m1[jb].key), ya_nat,
                     V(ga[jb].t[:, :].rearrange("p (n t) -> p n t", t=8), ga[jb].key), ALU.mult)
                o.tt("dve", m2[jb][:], p3[:], gb[jb][:], ALU.mult)
                o.tt("dve", mT.sub((slice(None), j, slice(None)), "post_mT%d" % j), m1[jb][:], m2[jb][:], ALU.add)
            for s_ in range(4):
                for half in range(2):
                    ph = nps()
                    for j in range(8):
                        o.mm(ph[:], V(mT.t[:, j, s_ * 128:(s_ + 1) * 128], "post_mT%d" % j),
                             V(wo.t[:, j, half * 512:(half + 1) * 512], wok[j]), start=(j == 0), stop=(j == 7))
                    o.tt("dve", V(xt.t[:, s_, half * 512:(half + 1) * 512], "post_h%d" % s_), ph[:],
                         V(xt.t[:, s_, half * 512:(half + 1) * 512], xt.key), ALU.add)
                hv = V(xt.t[:, s_, :], "post_h%d" % s_)
                r0 = tt * 512 + s_ * 128
                o.dma("sp", "post_hst", V(io["h_d"][r0:r0 + 128, :], "h_d"), hv)
                o.act(hsq[:], hv, AF.Square, accum=hss[:, s_:s_ + 1])
                o.ts("dve", hss[:, s_:s_ + 1], hss[:, s_:s_ + 1], 1.0 / D, 1e-6, ALU.mult, ALU.add)
                o.act(hss[:, s_:s_ + 1], hss[:, s_:s_ + 1], AF.Sqrt)
                kb.op("dve", lambda g_: g_.reciprocal(hss.t[:, s_:s_ + 1], hss.t[:, s_:s_ + 1]), [hss.key], [hss.key])
                o.stt(hn[:], hv, hss[:, s_:s_ + 1], fnw[:], ALU.mult, ALU.mult)
                for dc in range(8):
                    pt = nps()
                    o.tr(pt[:, 0:128], hn[:, dc * 128:(dc + 1) * 128], identf[:])
                    o.cp(kb.evac_engine(), hnT.sub((slice(None), dc, slice(s_ * 128, (s_ + 1) * 128)), "post_hnT"),
                         pt[:, 0:128])
            o.dma("sp", "post_hnst", V(io["hnT_d"].rearrange("(j p) t -> p j t", p=128)[:, :, tk], "hnT_d"),
                  V(hnT.t[:], "post_hnT"))


def peer_prepass(kb, io, P):
    o = OPS(kb)
    with kb.scope():
        identf = Tl(kb.ident, "ident")
        ust = [o.tile("pp_ust%d" % i, [128, 1024], F32) for i in range(2)]
        vst = [o.tile("pp_vst%d" % i, [128, 1024], F32) for i in range(2)]
        uTs = [o.tile("pp_uTs%d" % i, [128, 8, 128], BF16) for i in range(2)]
        vb = [o.tile("pp_vb%d" % i, [128, 1024], BF16) for i in range(2)]
        pu = [o.ptile("pp_pu%d" % i, [128, 8, 128], F32) for i in range(2)]
        uTv = io["uT_d"].rearrange("(j p) e -> p j e", p=128)
        for et in range(128):
            b = et % 2
            rows = slice(et * 128, (et + 1) * 128)
            o.dma("sp", "pp_ul%d" % b, ust[b][:], V(io["peer_u"][rows, :], "in"))
            o.dma("act", "pp_vl%d" % b, vst[b][:], V(io["peer_v"][rows, :], "in"))
            for dc in range(8):
                o.tr(pu[b][:, dc, :], ust[b][:, dc * 128:(dc + 1) * 128], identf[:], noinc=(dc < 7), noself=(dc > 0))
            o.cp("act", uTs[b][:], pu[b][:])
            o.dma("sp", "pp_ust", V(uTv[:, :, rows], "uT_d"), uTs[b][:])
            o.cp("dve" if b == 0 else "pool", vb[b][:], vst[b][:])
            o.dma("act", "pp_vst", V(io["v_d"][rows, :], "v_d"), vb[b][:])


def peer_phase(kb, io, P, dbg=False):
    o = OPS(kb)
    NEGB = -1.0e30
    with kb.scope():
        identf = Tl(kb.ident, "ident")
        wq = load_w_bf16(kb, o, "wq", io["peer_w_q"], 1024, 2048)
        wqk = ["wq_%d" % t for t in range(8)]
        keysT = o.tile("pk_keysT", [128, 16, 128], BF16)
        with kb.scope():
            kst = o.tile("pk_kst", [128, 16, 128], F32)
            o.dma("sp", "pkld", kst[:], V(io["keysT_in"][0:128, :, :], "in"))
            o.cp("dve", keysT[:], kst[:])
        iota128 = o.tile("pk_iota128", [128, 128], F32)
        iota16 = o.tile("pk_iota16", [128, 16], F32)
        fnw = o.tile("pk_fnw", [128, 1024], F32)
        o.dma("sp", "pkld", iota128[:], V(io["iota128_bc"][:, :], "in"))
        o.dma("sp", "pkld", iota16[:], V(io["iota128_bc"][:, 0:16], "in"))
        o.dma("sp", "pkld", fnw[:], V(io["final_nw_bc"][:, :], "in"))
        hnT = o.tile("pk_hnT", [128, 8, 256], BF16)
        qT = o.tile("pk_qT", [128, 16, 256], BF16)
        sc = o.tile("pk_sc", [128, 2048], F32)
        sc2 = o.tile("pk_sc2", [128, 2048], F32)
        v8 = o.tile("pk_v8", [128, 16, 16], F32)
        ix = o.tile("pk_ix", [128, 16, 16], U32)
        ixf = o.tile("pk_ixf", [128, 16, 16], F32)
        cand = o.tile("pk_cand", [128, 8, 256], F32)
        bs = o.tile("pk_bs", [128, 8, 16], F32)
        bc = o.tile("pk_bc", [128, 8, 16], U32)
        au = o.tile("pk_au", [128, 8, 16], U32)
        bu = o.tile("pk_bu", [128, 8, 16], U32)
        af = o.tile("pk_af", [128, 8, 16], F32)
        bf = o.tile("pk_bf", [128, 8, 16], F32)
        oh = o.tile("pk_oh", [128, 8, 16, 16], F32)
        i1 = o.tile("pk_i1", [128, 128], F32)
        i2 = o.tile("pk_i2", [128, 128], F32)
        gg = o.tile("pk_gg", [128, 128], F32)
        gsum = o.tile("pk_gsum", [128, 8], F32)
        i1T = o.tile("pk_i1T", [128, 256], F32)
        i2T = o.tile("pk_i2T", [128, 256], F32)
        gT_ = o.tile("pk_gT", [128, 256], F32)
        Pm = o.tile("pk_P", [128, 32, 128], BF16)
        Qm = o.tile("pk_Q", [128, 32, 128], BF16)
        Gd = o.tile("pk_Gd", [128, 256, 128], BF16)
        uTt = [o.tile("pk_uTt%d" % i, [128, 8, 256], BF16) for i in range(2)]
        vt = [o.tile("pk_vt%d" % i, [128, 2, 1024], BF16) for i in range(2)]
        gh = [o.tile("pk_gh%d" % i, [128, 512], BF16) for i in range(2)]
        ac = [o.tile("pk_ac%d" % i, [128, 512], BF16) for i in range(2)]
        hfin = Tl(oh.t[:].rearrange("p a b c -> p (a b c)")[:, 0:1024], oh.key)
        hsq = o.tile("pk_hsq", [128, 1024], BF16)
        hss = o.tile("pk_hss", [128, 1], F32)
        pA = o.ptile("pk_pA", [128, 2048], F32)
        pOut = o.ptile("pk_pOut", [128, 2, 1024], F32)
        uTv = io["uT_d"].rearrange("(j p) e -> p j e", p=128)
        hnTv = io["hnT_d"].rearrange("(j p) t -> p j t", p=128)

        def pa(bank, w=512):
            return V(pA.t[:, bank * 512:bank * 512 + w], "pA_b%d" % bank)

        for blk in range(16):
            t0 = blk * 256
            o.dma("sp", "pk_hl", hnT[:], V(hnTv[:, :, t0:t0 + 256], "hnT_d"))
            for k16 in range(16):
                pq = pa(k16 % 4, 256)
                for dc in range(8):
                    o.mm(pq, V(wq.t[:, dc, k16 * 128:(k16 + 1) * 128], wqk[dc]), hnT[:, dc, :],
                         start=(dc == 0), stop=(dc == 7))
                o.cp(kb.evac_engine(), qT.sub((slice(None), k16, slice(None)), "pk_qT%d" % k16), pq)
            for sub in range(2):
                ts_ = slice(sub * 128, (sub + 1) * 128)
                for k16 in range(16):
                    o.mm(V(pA.t[:, k16 * 128:(k16 + 1) * 128], "pA_b%d" % (k16 // 4)),
                         V(qT.t[:, k16, ts_], "pk_qT%d" % k16), keysT[:, k16, :])
                for bnk in range(4):
                    o.cp(kb.evac_engine(), sc.sub((slice(None), slice(bnk * 512, (bnk + 1) * 512)), "pk_sc%d" % bnk),
                         pa(bnk))
                for hs in range(16):
                    sk = "pk_sc%d" % (hs // 4)
                    scv = V(sc.t[:, hs * 128:(hs + 1) * 128], sk)
                    sc2v = V(sc2.t[:, hs * 128:(hs + 1) * 128], "pk_sc2_%d" % hs)
                    va = V(v8.t[:, hs, 0:8], "pk_v8a%d" % hs)
                    vb_ = V(v8.t[:, hs, 8:16], "pk_v8b%d" % hs)
                    kb.op("dve", lambda g: g.max(va.ap, scv.ap), [sk], [va.key])
                    kb.op("dve", lambda g: g.match_replace(sc2v.ap, va.ap, scv.ap, NEGB), [sk, va.key], [sc2v.key],
                          multi=True)
                    kb.op("dve", lambda g: g.max(vb_.ap, sc2v.ap), [sc2v.key], [vb_.key])
                    kb.op("dve", lambda g: g.max_index(ix.t[:, hs, 0:8], va.ap, scv.ap), [sk, va.key], ["pk_ix"],
                          multi=True)
                    kb.op("dve", lambda g: g.max_index(ix.t[:, hs, 8:16], vb_.ap, scv.ap), [sk, vb_.key], ["pk_ix"],
                          multi=True)
                v8keys = ["pk_v8a%d" % i for i in range(16)] + ["pk_v8b%d" % i for i in range(16)]
                o.cp("dve", ixf[:], V(ix.t[:], "pk_ix"))
                v84 = v8.t[:].rearrange("p (h s) k -> p h s k", s=2)
                kb.op("dve", lambda g: g.tensor_tensor(
                    cand.t[:].rearrange("p h (a b) -> p h a b", b=16),
                    v84[:, :, 0, :].unsqueeze(3).to_broadcast([128, 8, 16, 16]),
                    v84[:, :, 1, :].unsqueeze(2).to_broadcast([128, 8, 16, 16]), ALU.add), v8keys, ["pk_cand"])
                for h in range(8):
                    cv = V(cand.t[:, h, :], "pk_cand")
                    c2 = V(sc2.t[:, h * 256:(h + 1) * 256], "pk_sc2_%d" % (2 * h))
                    kb.op("dve", lambda g: g.max(bs.t[:, h, 0:8], cv.ap), ["pk_cand"], ["pk_bs%d" % h])
                    c2k = ["pk_sc2_%d" % (2 * h), "pk_sc2_%d" % (2 * h + 1)]
                    kb.op("dve", lambda g: g.match_replace(c2.ap, bs.t[:, h, 0:8], cv.ap, NEGB),
                          ["pk_cand", "pk_bs%d" % h], c2k, multi=True)
                    kb.op("dve", lambda g: g.max(bs.t[:, h, 8:16], c2.ap), c2k, ["pk_bsb%d" % h])
                    kb.op("dve", lambda g: g.max_index(bc.t[:, h, 0:8], bs.t[:, h, 0:8], cv.ap),
                          ["pk_cand", "pk_bs%d" % h], ["pk_bc"], multi=True)
                    kb.op("dve", lambda g: g.max_index(bc.t[:, h, 8:16], bs.t[:, h, 8:16], cv.ap),
                          ["pk_cand", "pk_bsb%d" % h], ["pk_bc"], multi=True)
                bskeys = ["pk_bs%d" % h for h in range(8)] + ["pk_bsb%d" % h for h in range(8)]
                bcv = V(bc.t[:], "pk_bc")
                o.ts("dve", au[:], bcv, 4, None, ALU.logical_shift_right)
                o.ts("dve", bu[:], bcv, 15, None, ALU.bitwise_and)
                o.cp("dve", af[:], au[:])
                o.cp("dve", bf[:], bu[:])
                ixf4 = ixf.t[:].rearrange("p (h s) k -> p h s k", s=2)
                for which, (sel_f, outt) in enumerate([(af, i1), (bf, i2)]):
                    kb.op("dve", lambda g: g.tensor_tensor(
                        oh.t[:], sel_f.t[:].unsqueeze(3).to_broadcast([128, 8, 16, 16]),
                        iota16.t[:, :].unsqueeze(1).unsqueeze(1).to_broadcast([128, 8, 16, 16]), ALU.is_equal),
                        [sel_f.key, iota16.key], [oh.key])
                    kb.op("dve", lambda g: g.tensor_tensor(
                        oh.t[:], oh.t[:], ixf4[:, :, which, :].unsqueeze(2).to_broadcast([128, 8, 16, 16]), ALU.mult),
                        [oh.key, ixf.key], [oh.key])
                    kb.op("dve", lambda g: g.tensor_reduce(outt.t[:, :], oh.t[:].rearrange("p h k a -> p (h k) a"),
                                                            AX.X, ALU.add), [oh.key], [outt.key])
                bsv = V(bs.t[:], "pk_bsall")
                kb.op("dve", lambda g: g.tensor_tensor(af.t[:], bs.t[:], bs.t[:, :, 0:1].to_broadcast([128, 8, 16]),
                                                        ALU.subtract), bskeys + [af.key], [af.key])
                o.act(af[:], af[:], AF.Exp)
                kb.op("dve", lambda g: g.tensor_reduce(gsum.t[:, :], af.t[:], AX.X, ALU.add), [af.key], [gsum.key])
                kb.op("dve", lambda g: g.reciprocal(gsum.t[:, :], gsum.t[:, :]), [gsum.key], [gsum.key])
                kb.op("dve", lambda g: g.tensor_tensor(gg.t[:, :].rearrange("p (h k) -> p h k", k=16), af.t[:],
                                                        gsum.t[:, :].unsqueeze(2).to_broadcast([128, 8, 16]), ALU.mult),
                      [af.key, gsum.key], [gg.key])
                if dbg and blk == 0 and sub == 0:
                    o.dma("sp", "dbgo", V(io["dbg_i1"][:, :], "dbg_i1"), i1[:])
                    o.dma("sp", "dbgo", V(io["dbg_i2"][:, :], "dbg_i2"), i2[:])
                    o.dma("sp", "dbgo", V(io["dbg_g"][:, :], "dbg_g"), gg[:])
                for n_, (src, dstT) in enumerate([(i1, i1T), (i2, i2T), (gg, gT_)]):
                    pt = V(pA.t[:, n_ * 512:n_ * 512 + 128], "pA_b%d" % n_)
                    o.tr(pt, src[:], identf[:])
                    o.cp(kb.evac_engine(), dstT[:, ts_], pt)
            for tg in range(8):
                tsl = slice(tg * 32, (tg + 1) * 32)
                io3 = V(iota128.t[:, :].unsqueeze(1).to_broadcast([128, 32, 128]), iota128.key)
                o.tt("dve", Pm[:], io3, V(i1T.t[:, tsl].unsqueeze(2).to_broadcast([128, 32, 128]), i1T.key), ALU.is_equal)
                o.tt("dve", Pm[:], Pm[:], V(gT_.t[:, tsl].unsqueeze(2).to_broadcast([128, 32, 128]), gT_.key), ALU.mult)
                o.tt("dve", Qm[:], io3, V(i2T.t[:, tsl].unsqueeze(2).to_broadcast([128, 32, 128]), i2T.key), ALU.is_equal)
                for t4 in range(8):
                    bnk = t4 % 2
                    for q in range(4):
                        t = t4 * 4 + q
                        o.mm(V(pA.t[:, bnk * 512 + q * 128:bnk * 512 + (q + 1) * 128], "pA_b%d" % bnk),
                             Qm[:, t, :], Pm[:, t, :], noinc=(q < 3), noself=(q > 0))
                    tb = tg * 32 + t4 * 4
                    o.cp(kb.evac_engine(), V(Gd.t[:, tb:tb + 4, :].rearrange("p a b -> p (a b)"), "pk_Gd%d" % (tb // 64)),
                         pa(bnk))
            gdkeys = ["pk_Gd%d" % i for i in range(4)]
            vdv = io["v_d"].rearrange("(e p) d -> p e d", p=128)
            for ep in range(SWEEP_ET // 2):
                b3 = ep % 2
                cols = slice(ep * 256, (ep + 1) * 256)
                o.dma("sp", "pk_ul%d" % b3, uTt[b3][:], V(uTv[:, :, cols], "uT_d"))
                o.dma("act", "pk_vl%d" % b3, vt[b3][:], V(vdv[:, 2 * ep:2 * ep + 2, :], "v_d"))
                b2 = ep % 2
                ph = pa(2 + b2, 512)
                for e2 in range(2):
                    for dc in range(8):
                        o.mm(V(pA.t[:, (2 + b2) * 512 + e2 * 256:(2 + b2) * 512 + (e2 + 1) * 256], "pA_b%d" % (2 + b2)),
                             uTt[b3][:, dc, e2 * 128:(e2 + 1) * 128], hnT[:, dc, :], start=(dc == 0), stop=(dc == 7),
                             noinc=not (dc == 7 and e2 == 1), noself=not (dc == 0 and e2 == 0))
                o.act(gh[b2][:], ph, AF.Gelu)
                kb.op("dve", lambda g: g.tensor_tensor(ac[b2].t[:, :].rearrange("p (e t) -> p e t", e=2),
                                                        gh[b2].t[:, :].rearrange("p (e t) -> p e t", e=2),
                                                        Gd.t[:, :, 2 * ep:2 * ep + 2].rearrange("p t e -> p e t"), ALU.mult),
                      [gh[b2].key] + gdkeys, [ac[b2].key])
                for e2 in range(2):
                    et = 2 * ep + e2
                    for sub in range(2):
                        for half in range(2):
                            o.mm(V(pOut.t[:, sub, half * 512:(half + 1) * 512], "pk_pOut%d%d" % (sub, half)),
                                 ac[b2][:, e2 * 256 + sub * 128:e2 * 256 + (sub + 1) * 128],
                                 vt[b3][:, e2, half * 512:(half + 1) * 512],
                                 start=(et == 0), stop=(et == SWEEP_ET - 1))
            for sub in range(2):
                r0 = t0 + sub * 128
                o.dma("sp", "pk_hld", hfin[:], V(io["h_d"][r0:r0 + 128, :], "h_d"))
                for half in range(2):
                    hs_ = slice(half * 512, (half + 1) * 512)
                    o.tt("dve", V(hfin.t[:, hs_], hfin.key), V(hfin.t[:, hs_], hfin.key),
                         V(pOut.t[:, sub, hs_], "pk_pOut%d%d" % (sub, half)), ALU.add)
                o.act(hsq[:], hfin[:], AF.Square, accum=hss[:, 0:1])
                o.ts("dve", hss[:], hss[:], 1.0 / D, 1e-6, ALU.mult, ALU.add)
                o.act(hss[:], hss[:], AF.Sqrt)
                kb.op("dve", lambda g_: g_.reciprocal(hss.t[:, :], hss.t[:, :]), [hss.key], [hss.key])
                o.stt(hfin[:], hfin[:], hss[:, 0:1], fnw[:], ALU.mult, ALU.mult)
                o.dma("sp", "pk_yst", V(io["y"][r0:r0 + 128, :], "y"), hfin[:])


def phase_a(kb, io, dbg):
    nc = kb.nc
    x_ext, w_in, nw_pc = io["x_ext"], io["w_in"], io["nw_pc"]
    ident = kb.ident
    w_bf = kb.sb("w_bf", [128, 8, INW], BF16)
    nw = kb.sb("nw", [128, 8], F32)
    kb.dma("sp", "nw", nw[:], nw_pc[:, :], writes=["nw"])
    HW = INW // 2
    stg = [kb.sb("wstg0", [128, HW], F32), kb.sb("wstg1", [128, HW], F32)]
    for dc in range(8):
        for hf in range(2):
            s = stg[hf]
            sk = "wstg%d" % hf
            kb.dma("sp", sk, s[:], w_in[dc * 128:(dc + 1) * 128, hf * HW:(hf + 1) * HW], writes=[sk])
            e = "dve" if hf == 0 else "pool"
            kb.op(e, lambda g: g.tensor_scalar(w_bf[:, dc, hf * HW:(hf + 1) * HW], s[:], nw[:, dc:dc + 1], None,
                                               ALU.mult),
                  reads=[sk, "nw"], writes=["w_bf%d_%d" % (dc, hf)])
    wkeys = [["w_bf%d_0" % dc, "w_bf%d_1" % dc] for dc in range(8)]

    xt = [kb.sb("xt0", [128, 4, D], F32), kb.sb("xt1", [128, 4, D], F32)]
    xn = xt
    xnT = [kb.sb("xnT0", [128, 8, 512], BF16), kb.sb("xnT1", [128, 8, 512], BF16)]
    sq = kb.sb("sqjunk", [128, D], BF16)
    ss = kb.sb("ss", [128, 8], F32)
    rstd = kb.sb("rstd", [128, 8], F32)
    evs = [kb.sb("evs%d" % i, [128, 512], BF16) for i in range(4)]
    psb = kb.psb
    dt_raw = kb.dt_raw
    pi = [0]

    def nextps():
        pi[0] = (pi[0] + 1) % 8
        return pi[0]

    ei = [0]

    def nextev():
        ei[0] = (ei[0] + 1) % 4
        return ei[0]

    for tt in range(9):
        b = tt % 2
        halo = (tt == 8)
        nsub = 1 if halo else 4
        ntok = 4 if halo else 512
        xk, xnk, xtk = "xt%d" % b, "xt%d" % b, "xnT%d" % b
        if not halo:
            src = x_ext[2 + tt * 512: 2 + (tt + 1) * 512, :].rearrange("(s p) d -> p s d", p=128)
            kb.dma("sp", xk, xt[b][:], src, writes=[xk])
        else:
            kb.dma("sp", xk, xt[b][0:2, 0, :], x_ext[0:2, :], writes=[xk])
            kb.dma("sp", xk, xt[b][2:4, 0, :], x_ext[T + 2:T + 4, :], writes=[xk])
        np_ = 4 if halo else 128
        for s in range(nsub):
            col = b * 4 + s
            kb.op("act", lambda g: g.activation(sq[0:np_, :], xt[b][0:np_, s, :], AF.Square,
                                                accum_out=ss[0:np_, col:col + 1]),
                  reads=[xk], writes=["sq", "ss%d" % col], multi=True)
            kb.op("dve", lambda g: g.tensor_scalar(rstd[0:np_, col:col + 1], ss[0:np_, col:col + 1],
                                                   1.0 / D, 1e-6, ALU.mult, ALU.add),
                  reads=["ss%d" % col], writes=["rs%d" % col])
            kb.op("act", lambda g: g.activation(rstd[0:np_, col:col + 1], rstd[0:np_, col:col + 1], AF.Sqrt),
                  reads=["rs%d" % col], writes=["rs%d" % col])
            kb.op("dve", lambda g: g.reciprocal(rstd[0:np_, col:col + 1], rstd[0:np_, col:col + 1]),
                  reads=["rs%d" % col], writes=["rs%d" % col])
            kb.op("dve", lambda g: g.tensor_scalar(xn[b][0:np_, s, :], xt[b][0:np_, s, :],
                                                   rstd[0:np_, col:col + 1], None, ALU.mult),
                  reads=[xk, "rs%d" % col], writes=[xk])
            for dc in range(8):
                p = nextps()
                pk = "ps%d" % p
                pt = kb.psb[p]
                kb.tr(pt[:, 0:np_], xn[b][0:np_, s, dc * 128:(dc + 1) * 128], ident[0:np_, 0:np_],
                      reads=[xk, "ident"], writes=[pk])
                kb.copy(kb.evac_engine(), xnT[b][:, dc, s * 128:s * 128 + np_], pt[:, 0:np_],
                        reads=[pk], writes=[xtk + "_%d" % dc])
        xtkeys = [xtk + "_%d" % dc for dc in range(8)]
        if halo:
            coltiles = [("x", j) for j in range(12)]
        else:
            coltiles = [("u", j) for j in range(4)] + [("x", j) for j in range(12)] + [("g", j) for j in range(16)]
        for kind, j in coltiles:
            c0 = {"u": C_U, "x": C_X, "g": C_GA}[kind] + j * 128
            p = nextps()
            pk = "ps%d" % p
            for dc in range(8):
                kb.mm(psb[p][:, 0:ntok], w_bf[:, dc, c0:c0 + 128], xnT[b][:, dc, 0:ntok], dc == 0, dc == 7,
                      reads=wkeys[dc] + [xtkeys[dc]], writes=[pk])
            if kind == "u":
                ev = nextev()
                ek = "evs%d" % ev
                dst = evs[ev][:, :].rearrange("p (s j) -> p s j", s=8)
                srcp = psb[p][:, :].rearrange("p (j s) -> p s j", s=8)
                kb.copy(kb.evac_engine(), dst, srcp, reads=[pk], writes=[ek])
                kb.dma("sp", "st_a", io["uT2d"][j * 128:(j + 1) * 128, :, tt * 64:(tt + 1) * 64],
                       evs[ev][:, :].rearrange("p (s j) -> p s j", s=8), reads=[ek], writes=["uT2d"])
            elif kind == "x":
                ev = nextev()
                ek = "evs%d" % ev
                kb.copy(kb.evac_engine(), evs[ev][:, 0:ntok], psb[p][:, 0:ntok], reads=[pk], writes=[ek])
                if halo:
                    kb.dma("sp", "st_a", io["xbcT"][j * 128:(j + 1) * 128, 0:2], evs[ev][:, 0:2],
                           reads=[ek], writes=["xbcT"])
                    kb.dma("sp", "st_a", io["xbcT"][j * 128:(j + 1) * 128, T + 2:T + 4], evs[ev][:, 2:4],
                           reads=[ek], writes=["xbcT"])
                else:
                    kb.dma("sp", "st_a", io["xbcT"][j * 128:(j + 1) * 128, 2 + tt * 512:2 + (tt + 1) * 512],
                           evs[ev][:, :], reads=[ek], writes=["xbcT"])
            else:
                ev = nextev()
                ek = "evs%d" % ev
                kb.op("act", lambda g: g.activation(evs[ev][:, :], psb[p][:, :], AF.Sigmoid),
                      reads=[pk], writes=[ek])
                kb.dma("sp", "st_a", io["gT"][j * 128:(j + 1) * 128, tt * 512:(tt + 1) * 512], evs[ev][:, :],
                       reads=[ek], writes=["gT"])
        if halo:
            continue
        for s in range(4):
            for half in range(2):
                p = nextps()
                pk = "ps%d" % p
                c0 = C_Z + half * 512
                for dc in range(8):
                    kb.mm(psb[p][:, :], xnT[b][:, dc, s * 128:(s + 1) * 128], w_bf[:, dc, c0:c0 + 512],
                          dc == 0, dc == 7, reads=wkeys[dc] + [xtkeys[dc]], writes=[pk])
                ev = nextev()
                ek = "evs%d" % ev
                kb.op("act", lambda g: g.activation(evs[ev][:, :], psb[p][:, :], AF.Silu),
                      reads=[pk], writes=[ek])
                r0 = tt * 512 + s * 128
                kb.dma("sp", "st_a", io["zs"][r0:r0 + 128, half * 512:(half + 1) * 512], evs[ev][:, :],
                       reads=[ek], writes=["zs"])
            p = nextps()
            pk = "ps%d" % p
            for dc in range(8):
                kb.mm(psb[p][:, 0:32], xnT[b][:, dc, s * 128:(s + 1) * 128], w_bf[:, dc, C_DT:C_DT + 32],
                      dc == 0, dc == 7, reads=wkeys[dc] + [xtkeys[dc]], writes=[pk])
            kb.copy("dve", dt_raw[:, tt * 4 + s, :], psb[p][:, 0:32], reads=[pk], writes=["dt_raw"])


def build_nc(dbg=False, phases=("s5", "ssd", "post", "peer")):
    nc = bass.Bass("TRN2", target_bir_lowering=False)
    skind = "ExternalOutput" if dbg else "Internal"
    io = {}

    def din(name, shape, dt=F32):
        io[name] = nc.dram_tensor(name, list(shape), dt, kind="ExternalInput").ap()

    def dscr(name, shape, dt):
        io[name] = nc.dram_tensor(name, list(shape), dt, kind=skind).ap()

    def dout(name, shape, dt):
        io[name] = nc.dram_tensor(name, list(shape), dt, kind="ExternalOutput").ap()

    din("x_ext", [T + 4, D])
    din("w_in", [D + 1, INW])
    din("nw_pc", [128, 8])
    din("ident_in", [128, 128])
    din("s5_par", [2 + 1, 128, 3, 2048])
    din("s5_B", [2 + 1, 128, 2, 2048])
    din("s5_C", [2 + 1, 128, 2, 2048])
    din("s5_ecol", [128, 6])
    din("s5_msk", [128, 3, 128])
    din("s5_dsk", [128, 32])
    dscr("zs", [T, 1024], BF16)
    dscr("xbcT", [1536, T + 4], BF16)
    dscr("gT", [2048, T], BF16)
    dout("y", [T, D], F32)

    din("conv_w_pc", [128, 12, 5])
    din("conv_b_pc", [128, 12])
    din("ssd_cst", [128, 80])
    din("ssd_nw_bc", [128, 1024])
    din("ssd_mats", [128, 5, 128])
    dscr("x_tm", [T, 1024], BF16)
    dscr("B_tm", [T, 256], BF16)
    dscr("ynT", [1024, T], BF16)
    cc_ssd_src_t = nc.dram_tensor("cc_ssd_src", [128, 1024], F32)
    cc_ssd_dst_t = nc.dram_tensor("cc_ssd_dst", [8 * 128, 1024], F32)
    din("w_glu", [512 + 1, 2048])
    din("w_ssd_out", [1024 + 1, 1024])
    din("w_o", [1024 + 1, 1024])
    din("ffn_nw_bc", [128, 1024])
    dscr("h_d", [T, D], F32)
    dscr("hnT_d", [D, T], BF16)
    din("peer_w_q", [1024 + 1, 2048])
    din("keysT_in", [128 + 1, 16, 128])
    din("iota128_bc", [128, 128])
    din("final_nw_bc", [128, 1024])
    din("peer_u", [16384 + 1, 1024])
    din("peer_v", [16384 + 1, 1024])
    dscr("uT_d", [1024, 16384], BF16)
    dscr("v_d", [16384, 1024], BF16)
    if dbg:
        dout("dbg_i1", [128, 128], F32)
        dout("dbg_i2", [128, 128], F32)
        dout("dbg_g", [128, 128], F32)
    din("s5_par2", [2, 128, 3, 16])
    din("miota", [128, 512])
    din("sel", [128, 16])
    dscr("ys5P", [512, 8, 512], BF16)
    dscr("uT2d", [512, 8, 512], BF16)
    cc_s5_src_t = nc.dram_tensor("cc_s5_src", [128, 64], F32)
    cc_s5_dst_t = nc.dram_tensor("cc_s5_dst", [8 * 128, 64], F32)

    with ExitStack() as es:
        kb = KB(nc, es)
        o = OPS(kb)
        P = {"cc_s5_src_t": cc_s5_src_t, "cc_s5_dst_t": cc_s5_dst_t,
             "cc_s5_src": cc_s5_src_t.ap(), "cc_s5_dst": cc_s5_dst_t.ap(),
             "cc_ssd_src_t": cc_ssd_src_t, "cc_ssd_dst_t": cc_ssd_dst_t,
             "cc_ssd_src": cc_ssd_src_t.ap(), "cc_ssd_dst": cc_ssd_dst_t.ap()}
        kb.ident = kb.sb("ident", [128, 128], F32)
        kb.dma("sp", "ident", kb.ident[:], io["ident_in"][:, :], writes=["ident"])
        P["identb"] = o.tile("identb", [128, 128], BF16)
        o.cp("dve", P["identb"][:], V(kb.ident[:], "ident"))
        kb.dt_raw = kb.sb("dt_raw", [128, 32, 32], F32)
        with kb.scope():
            kb.psb = [kb.ps("psb%d" % i, [128, 512], F32) for i in range(8)]
            phase_a(kb, io, dbg)
        with kb.scope():
            P["s5tab"] = [[(o.tile("s5tab%d%d0" % (d, ti), [128, 2048], BF16),
                            o.tile("s5tab%d%d1" % (d, ti), [128, 2048], BF16)) for ti in range(1)] for d in range(2)]
            P["s5wout"] = [[o.tile("s5wout%d%d" % (d, c), [128, 16, 128], BF16) for c in range(2)] for d in range(2)]
            P["s5mintra"] = o.tile("s5mintra", [128, 32, 128], BF16)
            P["U"] = o.tile("U", [128, 32, 512], BF16)
            s5_shuffle_u(kb, io, P)
            phase_s5_setup(kb, io, P)
            if "s5" in phases:
                s5_main(kb, io, P)
        if "ssd" in phases:
            ssd_phase(kb, io, P)
        if "post" in phases:
            post_phase(kb, io, P)
        if "peer" in phases:
            peer_prepass(kb, io, P)
            peer_phase(kb, io, P, dbg)
        kb.barrier()
        print("instructions:", kb.ninst)
    return nc


def host_inputs(inputs):
    f = lambda k: np.asarray(inputs[k], np.float32)
    x = f("x")
    B, S, _ = x.shape
    maps = []
    ident = np.eye(128, dtype=np.float32)
    nw_pc = np.ascontiguousarray(f("mix_norm_w")[0].reshape(8, 128).T)
    w_in = np.ascontiguousarray(f("w_in")[0])
    a_re, a_im, ldt = f("s5_a_re")[0], f("s5_a_im")[0], f("s5_log_dt")[0]
    par = np.stack([a_re.reshape(2, 2048), a_im.reshape(2, 2048),
                    np.repeat(ldt, 64, axis=1)], axis=1)
    s5_par = np.ascontiguousarray(np.broadcast_to(par[:, None], (2, 128, 3, 2048)))
    def bT(b):
        t = np.transpose(b, (0, 3, 1, 2)).reshape(2, 16, 2048)
        return np.tile(t, (1, 8, 1))
    s5_B = np.ascontiguousarray(np.stack([bT(f("s5_b_re")[0]), bT(f("s5_b_im")[0])], axis=2))
    def cR(c):
        t = np.transpose(c, (0, 2, 1, 3)).reshape(2, 16, 2048)
        return np.tile(t, (1, 8, 1))
    s5_C = np.ascontiguousarray(np.stack([cR(f("s5_c_re")[0]), cR(f("s5_c_im")[0])], axis=2))
    xx = (np.arange(128) // 16).astype(np.float32)
    s5_ecol = np.stack([7 - xx, xx + 1, xx - 7, xx, 8 - xx, -xx], axis=1).astype(np.float32)
    cc = np.arange(128) % 16
    s_, t_ = xx[:, None], xx[None, :]
    s5_msk = np.stack([(t_ >= s_), (s_ >= t_), (s_ == t_) & (cc[:, None] == cc[None, :])], axis=1).astype(np.float32)
    s5_dsk = np.ascontiguousarray(np.tile(f("s5_d")[0].reshape(32, 16).T, (8, 1)))
    def sc(a):
        return np.transpose(a.reshape(2, 16, 2, 64), (0, 2, 3, 1)).reshape(2, 128, 16)
    s5_par2 = np.ascontiguousarray(np.stack([sc(a_re), sc(a_im), sc(np.repeat(ldt[:, :, None], 64, axis=2))], axis=2))
    miota = np.ascontiguousarray(np.broadcast_to(np.arange(512, dtype=np.float32)[None], (128, 512)))
    conv_w_pc = np.ascontiguousarray(np.transpose(f("conv_w")[0][:, 0, :].reshape(5, 12, 128), (2, 1, 0)))
    conv_b_pc = np.ascontiguousarray(f("conv_b")[0].reshape(12, 128).T)
    cst = np.concatenate([f("ssd_dt_bias")[0].reshape(32), f("ssd_a_log")[0].reshape(32), f("ssd_d")[0].reshape(16)])
    ssd_cst = np.ascontiguousarray(np.broadcast_to(cst[None], (128, 80)))
    ssd_nw_bc = np.ascontiguousarray(np.broadcast_to(f("ssd_norm_w")[0][None], (128, 1024)))
    ii = np.arange(128)
    le = (ii[:, None] <= ii[None, :])
    ge = (ii[:, None] >= ii[None, :])
    ssd_mats = np.ascontiguousarray(np.stack([le.astype(np.float32), ge.astype(np.float32),
                                              np.where(le, 0.0, -30000.0).astype(np.float32),
                                              np.where(ge, 0.0, -30000.0).astype(np.float32),
                                              np.ones((128, 128), np.float32)], axis=1))
    shared0 = {"w_glu": np.ascontiguousarray(f("w_glu")[0]), "w_ssd_out": np.ascontiguousarray(f("w_ssd_out")[0]),
               "w_o": np.ascontiguousarray(f("w_o")[0]),
               "ffn_nw_bc": np.ascontiguousarray(np.broadcast_to(f("ffn_norm_w")[0][None], (128, 1024)))}
    shared0.update({
        "peer_w_q": np.ascontiguousarray(f("peer_w_q")[0]),
        "keysT_in": np.ascontiguousarray(np.transpose(f("peer_sub_keys")[0].reshape(16, 128, 128), (2, 0, 1))),
        "iota128_bc": np.ascontiguousarray(np.broadcast_to(np.arange(128, dtype=np.float32)[None], (128, 128))),
        "final_nw_bc": np.ascontiguousarray(np.broadcast_to(f("final_norm_w")[None], (128, 1024))),
        "peer_u": np.ascontiguousarray(f("peer_u")[0]), "peer_v": np.ascontiguousarray(f("peer_v")[0])})
    shared = {"conv_w_pc": conv_w_pc, "conv_b_pc": conv_b_pc, "ssd_cst": ssd_cst, "ssd_nw_bc": ssd_nw_bc,
              "ssd_mats": ssd_mats, "s5_par2": s5_par2, "miota": miota, "w_in": w_in, "nw_pc": nw_pc, "ident_in": ident, "s5_par": s5_par, "s5_B": s5_B, "s5_C": s5_C,
              "s5_ecol": s5_ecol, "s5_msk": np.ascontiguousarray(s5_msk), "s5_dsk": s5_dsk}
    for c in range(NCORES):
        b, half = c // 2, c % 2
        t0 = half * T
        xe = np.zeros((T + 4, D), np.float32)
        lo, hi = max(t0 - 2, 0), min(t0 + T + 2, S)
        xe[lo - (t0 - 2): hi - (t0 - 2)] = x[b, lo:hi]
        m = dict(shared)
        m.update(shared0)
        for k_ in PADDED:
            a_ = m[k_]
            m[k_] = np.concatenate([a_, np.full((1,) + a_.shape[1:], float(c), np.float32)], axis=0)
        m["x_ext"] = xe
        sel = np.zeros((128, 16), np.float32)
        if half == 1:
            sel[:, c - 1] = 1.0
        else:
            sel[:, 8 + c + 1] = 1.0
        m["sel"] = sel
        maps.append(m)
    return maps


def kernel(**inputs):
    nc = build_nc(dbg=False)
    maps = host_inputs(inputs)
    res = run_bass_kernel_spmd(nc, maps, core_ids=list(range(NCORES)))
    out = np.zeros((4, 8192, D), np.float32)
    for c in range(NCORES):
        b, half = c // 2, c % 2
        out[b, half * T:(half + 1) * T] = res.results[c]["y"]
    return out
```
